# Optimizing a Trainium2 kernel written in Bass

```python
import jax, jax.numpy as jnp
from jax import lax
import numpy as np

D_MODEL = 1024
BATCH = 16
SEQ = 2048
DEPTH = 2

N_MIXERS = 2
NORM_EPS = 1e-6
NEG = -1e30
D_FF = 4 * D_MODEL
RET_HEADS = 4
RET_DK = D_MODEL // RET_HEADS
RET_DV = 2 * RET_DK
RET_CHUNK = 128
RET_THETA = 10000.0
RET_IN = 2 * RET_HEADS * RET_DK + 2 * RET_HEADS * RET_DV
MOBA_HEADS = 8
MOBA_DH = D_MODEL // MOBA_HEADS
MOBA_BLOCK = 256
MOBA_TOPK = 3
MOBA_QCHUNK = 8
ROPE_THETA = 500000.0
ROPE_DIM = MOBA_DH // 4
MOBA_IN = 3 * MOBA_HEADS * MOBA_DH

kernel_name = "hybrid_retention_moba_block"


def rms_norm(x, g):
    xf = x.astype(jnp.float32)
    y = xf * lax.rsqrt(jnp.mean(xf * xf, axis=-1, keepdims=True) + NORM_EPS)
    return (y * g.astype(jnp.float32)).astype(x.dtype)


def sq_relu_mlp(x, w_up, w_down):
    h = jax.nn.relu(x @ w_up)
    return (h * h) @ w_down


def retnet_rotary(x, pos):
    d = x.shape[-1]
    freq = 1.0 / (RET_THETA ** jnp.linspace(0.0, 1.0, d // 2, dtype=jnp.float32))
    ang = pos[:, None].astype(jnp.float32) * freq[None, :]
    cos, sin = jnp.cos(ang), jnp.sin(ang)
    x1 = x[..., 0::2].astype(jnp.float32)
    x2 = x[..., 1::2].astype(jnp.float32)
    y = jnp.stack([x1 * cos - x2 * sin, x1 * sin + x2 * cos], axis=-1)
    return y.reshape(x.shape).astype(x.dtype)


def partial_rope(x, pos):
    half = ROPE_DIM // 2
    inv = ROPE_THETA ** (-jnp.arange(0, ROPE_DIM, 2, dtype=jnp.float32) / ROPE_DIM)
    ang = pos[:, None].astype(jnp.float32) * inv[None, :]
    cos, sin = jnp.cos(ang), jnp.sin(ang)
    x1 = x[..., :half].astype(jnp.float32)
    x2 = x[..., half:ROPE_DIM].astype(jnp.float32)
    rot = jnp.concatenate([x1 * cos - x2 * sin, x2 * cos + x1 * sin], axis=-1)
    return jnp.concatenate([rot.astype(x.dtype), x[..., ROPE_DIM:]], axis=-1)


def retention(h, w_in, w_out):
    B, S, _ = h.shape
    H, DK, DV, L = RET_HEADS, RET_DK, RET_DV, RET_CHUNK
    C = S // L
    q, k, v, g = jnp.split(h @ w_in, [H * DK, 2 * H * DK, 2 * H * DK + H * DV], axis=-1)
    q = q.reshape(B, S, H, DK).transpose(0, 2, 1, 3)
    k = k.reshape(B, S, H, DK).transpose(0, 2, 1, 3)
    v = v.reshape(B, S, H, DV).transpose(0, 2, 1, 3)
    pos = jnp.arange(S)
    q = retnet_rotary(q, pos)
    k = retnet_rotary(k, pos) * (DK ** -0.5)

    log_gamma = jnp.log(1.0 - 2.0 ** (-5.0 - jnp.arange(H, dtype=jnp.float32)))
    idx = jnp.arange(L, dtype=jnp.float32)
    diff = idx[:, None] - idx[None, :]
    d_intra = jnp.where(diff >= 0, jnp.exp(log_gamma[:, None, None] * jnp.maximum(diff, 0.0)), 0.0)
    q_decay = jnp.exp(log_gamma[:, None] * (idx + 1.0))[None, :, :, None]
    k_decay = jnp.exp(log_gamma[:, None] * (L - 1.0 - idx))[None, :, :, None]
    chunk_decay = jnp.exp(log_gamma * L)[None, :, None, None]

    qc = q.reshape(B, H, C, L, DK)
    kc = k.reshape(B, H, C, L, DK)
    vc = v.reshape(B, H, C, L, DV)
    scores = jnp.einsum('bhcid,bhcjd->bhcij', qc, kc).astype(jnp.float32) * d_intra[None, :, None]
    o_intra = jnp.einsum('bhcij,bhcje->bhcie', scores, vc)

    def step(state, inp):
        q_c, k_c, v_c = inp
        cross = jnp.einsum('bhid,bhde->bhie', q_c, state) * q_decay
        state = state * chunk_decay + jnp.einsum('bhjd,bhje->bhde', k_c * k_decay, v_c)
        return state, cross

    init = jnp.zeros((B, H, DK, DV), jnp.float32)
    xs = (qc.transpose(2, 0, 1, 3, 4), kc.transpose(2, 0, 1, 3, 4), vc.transpose(2, 0, 1, 3, 4))
    _, o_cross = lax.scan(step, init, xs)
    o = (o_intra + o_cross.transpose(1, 2, 0, 3, 4)).reshape(B, H, S, DV)
    o = o * lax.rsqrt(jnp.mean(o * o, axis=-1, keepdims=True) + NORM_EPS)
    o = o.transpose(0, 2, 1, 3).reshape(B, S, H * DV)
    o = o * jax.nn.silu(g.astype(jnp.float32))
    return o.astype(h.dtype) @ w_out


def moba_attention(h, w_in, w_out):
    B, S, _ = h.shape
    H, Dh, BS, QC = MOBA_HEADS, MOBA_DH, MOBA_BLOCK, MOBA_QCHUNK
    nb = -(-S // BS)
    Sp = nb * BS
    K_SEL = min(MOBA_TOPK, nb)
    q, k, v = jnp.split(h @ w_in, 3, axis=-1)
    q = q.reshape(B, S, H, Dh).transpose(0, 2, 1, 3)
    k = k.reshape(B, S, H, Dh).transpose(0, 2, 1, 3)
    v = v.reshape(B, S, H, Dh).transpose(0, 2, 1, 3)
    pos = jnp.arange(S)
    q = partial_rope(q, pos) * (Dh ** -0.5)
    k = partial_rope(k, pos)
    pad = ((0, 0), (0, 0), (0, Sp - S), (0, 0))
    q, k, v = jnp.pad(q, pad), jnp.pad(k, pad), jnp.pad(v, pad)
    qb = q.reshape(B, H, nb, BS, Dh)
    kb = k.reshape(B, H, nb, BS, Dh)
    vb = v.reshape(B, H, nb, BS, Dh)

    causal = jnp.tril(jnp.ones((BS, BS), dtype=bool))
    s_own = jnp.einsum('bhnid,bhnjd->bhnij', qb, kb).astype(jnp.float32)
    s_own = jnp.where(causal, s_own, NEG)
    m_own = jnp.max(s_own, axis=-1)
    p_own = jnp.exp(s_own - m_own[..., None])
    l_own = jnp.sum(p_own, axis=-1)
    o_own = jnp.einsum('bhnij,bhnjd->bhnid', p_own, vb)
    m_own, l_own = m_own.reshape(B, H, Sp), l_own.reshape(B, H, Sp)
    o_own = o_own.reshape(B, H, Sp, Dh)

    k_mean = jnp.mean(kb.astype(jnp.float32), axis=3)
    gate = jnp.einsum('bhsd,bhnd->bhsn', q.astype(jnp.float32), k_mean)
    q_blk = jnp.arange(Sp) // BS
    past = jnp.arange(nb)[None, :] < q_blk[:, None]
    gate = jnp.where(past, gate, NEG)
    _, sel = lax.top_k(gate, K_SEL)
    valid = sel < q_blk[:, None]

    nq = Sp // QC
    q_ch = q.reshape(B, H, nq, QC, Dh).transpose(2, 0, 1, 3, 4)
    sel_ch = sel.reshape(B, H, nq, QC, K_SEL).transpose(2, 0, 1, 3, 4)
    val_ch = valid.reshape(B, H, nq, QC, K_SEL).transpose(2, 0, 1, 3, 4)
    bi = jnp.arange(B)[:, None, None, None]
    hi = jnp.arange(H)[None, :, None, None]

    def attend_selected(args):
        qc, sc, vc = args
        k_sel = kb[bi, hi, sc]
        v_sel = vb[bi, hi, sc]
        s = jnp.einsum('bhqd,bhqkjd->bhqkj', qc, k_sel).astype(jnp.float32)
        s = jnp.where(vc[..., None], s, NEG)
        m = jnp.max(s, axis=(3, 4))
        p = jnp.exp(s - m[..., None, None])
        l = jnp.sum(p, axis=(3, 4))
        o = jnp.einsum('bhqkj,bhqkjd->bhqd', p, v_sel)
        return m, l, o

    m_sel, l_sel, o_sel = lax.map(attend_selected, (q_ch, sel_ch, val_ch))
    m_sel = m_sel.transpose(1, 2, 0, 3).reshape(B, H, Sp)
    l_sel = l_sel.transpose(1, 2, 0, 3).reshape(B, H, Sp)
    o_sel = o_sel.transpose(1, 2, 0, 3, 4).reshape(B, H, Sp, Dh)

    m = jnp.maximum(m_own, m_sel)
    a = jnp.exp(m_own - m)
    b = jnp.exp(m_sel - m)
    o = (o_own * a[..., None] + o_sel * b[..., None]) / (l_own * a + l_sel * b)[..., None]
    o = o[:, :, :S].transpose(0, 2, 1, 3).reshape(B, S, H * Dh)
    return o.astype(h.dtype) @ w_out


def setup_inputs(seed: int = 0) -> dict:
    key = jax.random.key(seed)
    ks = jax.random.split(key, 32)
    f32 = jnp.float32

    def w(k, shape):
        return jax.random.normal(k, shape, f32) * (shape[0] ** -0.5)

    def gain(k):
        return 1.0 + 0.05 * jax.random.normal(k, (D_MODEL,), f32)

    return {
        "x": jax.random.normal(ks[0], (BATCH, SEQ, D_MODEL), f32),
        "ln_mix_pre_0": gain(ks[1]),
        "ln_mix_post_0": gain(ks[2]),
        "w_in_0": w(ks[3], (D_MODEL, RET_IN)),
        "w_out_0": w(ks[4], (RET_HEADS * RET_DV, D_MODEL)),
        "ln_mlp_pre_0": gain(ks[5]),
        "ln_mlp_post_0": gain(ks[6]),
        "w_up_0": w(ks[7], (D_MODEL, D_FF)),
        "w_down_0": w(ks[8], (D_FF, D_MODEL)),
        "ln_mix_pre_1": gain(ks[9]),
        "ln_mix_post_1": gain(ks[10]),
        "w_in_1": w(ks[11], (D_MODEL, MOBA_IN)),
        "w_out_1": w(ks[12], (MOBA_HEADS * MOBA_DH, D_MODEL)),
        "ln_mlp_pre_1": gain(ks[13]),
        "ln_mlp_post_1": gain(ks[14]),
        "w_up_1": w(ks[15], (D_MODEL, D_FF)),
        "w_down_1": w(ks[16], (D_FF, D_MODEL)),
    }


def reference(x, ln_mix_pre_0, ln_mix_post_0, w_in_0, w_out_0, ln_mlp_pre_0, ln_mlp_post_0, w_up_0, w_down_0,
              ln_mix_pre_1, ln_mix_post_1, w_in_1, w_out_1, ln_mlp_pre_1, ln_mlp_post_1, w_up_1, w_down_1):
    mixers = (retention, moba_attention)
    layers = (
        (ln_mix_pre_0, ln_mix_post_0, w_in_0, w_out_0, ln_mlp_pre_0, ln_mlp_post_0, w_up_0, w_down_0),
        (ln_mix_pre_1, ln_mix_post_1, w_in_1, w_out_1, ln_mlp_pre_1, ln_mlp_post_1, w_up_1, w_down_1),
    )
    h = x
    for i in range(DEPTH):
        pre, post, w_in, w_out, mpre, mpost, w_up, w_down = layers[i]
        h = h + rms_norm(mixers[i % N_MIXERS](rms_norm(h, pre), w_in, w_out), post)
        h = h + rms_norm(sq_relu_mlp(rms_norm(h, mpre), w_up, w_down), mpost)
    return h
```

```python
import numpy as np
from contextlib import ExitStack
import concourse.bass as bass
import concourse.mybir as mybir
from concourse.bass_utils import run_bass_kernel_spmd

F32 = mybir.dt.float32
BF16 = mybir.dt.bfloat16
AF = mybir.ActivationFunctionType
ALU = mybir.AluOpType
AX = mybir.AxisListType

S = 2048
D = 1024
NT = 16
EPS = 1e-6
NCORES = 8
BIG = 30000.0
ENGS = ("pe", "act", "dve", "pool", "sp")


class Buf:
    __slots__ = ("w", "r")

    def __init__(self):
        self.w = None
        self.r = {}


class Prog:
    ROT = 8000

    def __init__(self):
        self.ops = {e: [] for e in ENGS}
        self.seen = {e: {} for e in ENGS}
        self.gen = {e: 0 for e in ENGS}
        self.cnt = {e: 0 for e in ENGS}
        self.semkeys = {}
        self.dmacnt = {}
        self.last = {}

    def _deps(self, eng, reads, writes):
        need = {}
        seen = self.seen[eng]

        def add(chan, gv):
            if chan == "pe" and eng == "pe":
                return
            s = seen.get(chan)
            if s is not None and s >= gv:
                return
            cur = need.get(chan)
            if cur is None or cur < gv:
                need[chan] = gv

        for b in reads:
            if b.w is not None:
                add(b.w[0], b.w[1])
        for b in writes:
            if b.w is not None:
                add(b.w[0], b.w[1])
            for chan, gv in b.r.items():
                add(chan, gv)
        waits = []
        for chan, gv in need.items():
            seen[chan] = gv
            key = (chan, gv[0])
            self.semkeys[key] = True
            waits.append((key, gv[1]))
        return waits

    def _mark(self, chan, gv, reads, writes):
        for b in reads:
            cur = b.r.get(chan)
            if cur is None or cur < gv:
                b.r[chan] = gv
        for b in writes:
            b.w = (chan, gv)
            b.r = {}
        self.last[chan] = gv

    def op(self, eng, fn, reads=(), writes=()):
        waits = self._deps(eng, reads, writes)
        if self.cnt[eng] >= self.ROT:
            self.gen[eng] += 1
            self.cnt[eng] = 0
        self.cnt[eng] += 1
        gv = (self.gen[eng], self.cnt[eng])
        key = (eng, gv[0])
        self.semkeys[key] = True
        self.ops[eng].append((waits, fn, (key, 1)))
        self._mark(eng, gv, reads, writes)

    def dma(self, queue, chan, fn, reads=(), writes=()):
        waits = self._deps(queue, reads, writes)
        n = self.dmacnt.get(chan, 0) + 1
        self.dmacnt[chan] = n
        gv = (0, 16 * n)
        key = (chan, 0)
        self.semkeys[key] = True
        self.ops[queue].append((waits, fn, (key, 16)))
        self._mark(chan, gv, reads, writes)

    def barrier(self, final=False):
        for eng in ENGS:
            waits = []
            seen = self.seen[eng]
            for chan, gv in self.last.items():
                if chan == "pe" and eng == "pe":
                    continue
                if chan.startswith("cv_") and not final:
                    continue
                s = seen.get(chan)
                if s is not None and s >= gv:
                    continue
                seen[chan] = gv
                waits.append(((chan, gv[0]), gv[1]))
            if waits:
                self.ops[eng].append((waits, None, None))

    def emit(self, nc, es):
        sems = {}
        for i, key in enumerate(self.semkeys):
            sems[key] = es.enter_context(nc.semaphore("s%d" % i))
        block = es.enter_context(nc.Block())

        def run(e, ops):
            for waits, fn, inc in ops:
                for key, val in waits:
                    e.wait_ge(sems[key], val)
                if fn is not None:
                    ins = fn(e)
                    ins.then_inc(sems[inc[0]], inc[1])

        @block.tensor
        def _(e):
            run(e, self.ops["pe"])

        @block.scalar
        def _(e):
            run(e, self.ops["act"])

        @block.vector
        def _(e):
            run(e, self.ops["dve"])

        @block.gpsimd
        def _(e):
            run(e, self.ops["pool"])

        @block.sync
        def _(e):
            run(e, self.ops["sp"])


def build_program(nseq=2, stop=4, sub=3, nheads=4, ngroups=4):
    nc = bass.Bass("TRN2", target_bir_lowering=False)
    P = Prog()
    es = ExitStack()

    def din(name, shape, dt=F32):
        return nc.dram_tensor(name, list(shape), dt, kind="ExternalInput").ap()

    def dscr(name, shape):
        return nc.dram_tensor(name, list(shape), BF16, kind="Internal").ap()

    x_d = din("x", [nseq, S, D])
    out_d = nc.dram_tensor("out", [nseq, S, D], F32, kind="ExternalOutput").ap()
    w0h_d = din("w0h", [4, D, 1536])
    wo0_d = din("wo0", [2048, D])
    wu_d = [din("wu0", [8, D, 512]), din("wu1", [8, D, 512])]
    wd_d = [din("wd0", [8, 512, D]), din("wd1", [8, 512, D])]
    w1h_d = din("w1h", [8, D, 384])
    wo1_d = din("wo1", [D, D])
    g8_d = din("g8", [8, 128, D])
    cs0_d = din("cs0", [2, 128, S])
    cs1_d = din("cs1", [2, 128, S])
    cmask_d = din("cmask", [128, 4 * 128 + 4 * 128 + 4 + 2 * 256 + 64 + 8 + 8])
    cbf_d = din("cbf", [128, 256])
    w0h_s = dscr("w0h_s", [4, D, 1536])
    wo0_s = dscr("wo0_s", [2048, D])
    wu_s = [dscr("wu0_s", [8, D, 512]), dscr("wu1_s", [8, D, 512])]
    wd_s = [dscr("wd0_s", [8, 512, D]), dscr("wd1_s", [8, 512, D])]
    w1h_s = dscr("w1h_s", [8, D, 384])
    wo1_s = dscr("wo1_s", [D, D])

    def sb(name, shape, dt):
        return es.enter_context(nc.sbuf_tensor(name, list(shape), dt))

    RH = sb("RH", [128, 32768], BF16)
    RX = sb("RX", [128, 16384], BF16)
    RO = sb("RO", [128, 32768], BF16)
    RS = sb("RS", [128, 12288], BF16)
    gbuf = sb("gbuf", [128, 2, D], F32)
    stats = sb("stats", [128, 256], F32)
    junk = sb("junk", [128, D], BF16)
    cmask = sb("cmask_sb", [128, 4 * 128 + 4 * 128 + 4 + 2 * 256 + 64 + 8 + 8], F32)
    cbf = sb("cbf_sb", [128, 256], BF16)
    PS = es.enter_context(nc.psum_tensor("PS", [128, 3072], F32))
    PSB = es.enter_context(nc.psum_tensor("PSB", [128, 2048], BF16))

    def view(reg, off_bytes, shape, dt):
        n = int(np.prod(shape))
        esz = 4 if dt == F32 else 2
        a = reg[:, off_bytes // 2: off_bytes // 2 + n * esz // 2]
        if dt == F32:
            a = a.bitcast(F32)
        if len(shape) == 2:
            a = a.rearrange("p (a b) -> p a b", a=shape[0])
        return a

    K = 1024
    maskT = cmask[:, 0:512].rearrange("p (h j) -> p h j", h=4)
    qdec = cmask[:, 512:1024].rearrange("p (h j) -> p h j", h=4)
    kdec = cmask[:, 1024:1028]
    causal = cmask[:, 1028:1540].rearrange("p (a j) -> p a j", a=2)
    pastm = cmask[:, 1540:1604].rearrange("p (b n) -> p b n", b=8)
    neghalf = cmask[:, 1604:1612]
    zeros8 = cmask[:, 1612:1620]
    ident = cbf[:, 0:128]
    Rmat = cbf[:, 128:256]

    PF = [PS[:, i * 512:(i + 1) * 512] for i in range(6)]
    PB = [PSB[:, i * 1024:(i + 1) * 1024] for i in range(2)]
    pfb = [Buf() for _ in range(6)]
    pbb = [Buf() for _ in range(2)]
    constb = Buf()

    hview = RH[:, :].bitcast(F32).rearrange("p (t d) -> p t d", t=NT)
    hb = [Buf() for _ in range(NT)]
    xnT = RX[:, :].rearrange("p (c t) -> p c t", c=8)
    xnb = [Buf() for _ in range(NT)]

    st_state = {"i": 0}
    stb = [Buf() for _ in range(32)]

    def stat():
        i = st_state["i"] % 32
        st_state["i"] += 1
        return stats[:, i * 8:(i + 1) * 8], stb[i]

    gb = [Buf(), Buf()]
    g_state = {"i": 0}

    def load_gain(idx):
        s = g_state["i"] % 2
        g_state["i"] += 1
        P.dma("sp", "gain%d" % s, lambda e, s=s, idx=idx: e.dma_start(out=gbuf[:, s, :], in_=g8_d[idx]),
              writes=[gb[s]])
        return gbuf[:, s, :], gb[s]

    cvb = {}

    def convert(name, src, dst, nchunk):
        b = Buf()
        cvb[name] = b
        tot = int(np.prod(src.shape))
        per = tot // nchunk
        sflat = src.rearrange(" ".join("abcd"[:src.ndim]) + " -> (" + " ".join("abcd"[:src.ndim]) + ")")
        dflat = dst.rearrange(" ".join("abcd"[:dst.ndim]) + " -> (" + " ".join("abcd"[:dst.ndim]) + ")")
        for c in range(nchunk):
            si = sflat[c * per:(c + 1) * per].rearrange("(p n) -> p n", p=128)
            di = dflat[c * per:(c + 1) * per].rearrange("(p n) -> p n", p=128)
            P.dma("pool", "cv_" + name, lambda e, si=si, di=di: e.dma_start(out=di, in_=si), writes=[b])

    def rstd_from_ss(ss_ap, ssb, n, out_ap, outb, tmp_ap):
        P.op("pool", lambda e: e.tensor_scalar(out=tmp_ap, in0=ss_ap, scalar1=1.0 / n, scalar2=EPS,
                                               op0=ALU.mult, op1=ALU.add), reads=[ssb], writes=[outb])
        P.op("pool", lambda e: e.tensor_tensor(out=out_ap, in0=tmp_ap, in1=neghalf[:, 0:1], op=ALU.pow),
             reads=[outb, constb], writes=[outb])

    def prenorm(tiles, src_fn, gain_ap, gain_b, dstT, dst_bufs, xs_view, xs_bufs):
        for n, i in enumerate(tiles):
            src, srcb = src_fn(i)
            st, stbuf = stat()
            P.op("act", lambda e, src=src, st=st: e.activation(out=junk[:, :], in_=src, func=AF.Square,
                                                                accum_out=st[:, 0:1]),
                 reads=[srcb], writes=[stbuf])
            rstd_from_ss(st[:, 0:1], stbuf, D, st[:, 2:3], stbuf, st[:, 1:2])
            xs = xs_view[:, n % 2, :]
            xsb = xs_bufs[n % 2]
            P.op("dve", lambda e, src=src, st=st, xs=xs: e.scalar_tensor_tensor(
                out=xs, in0=src, scalar=st[:, 2:3], in1=gain_ap, op0=ALU.mult, op1=ALU.mult),
                reads=[srcb, stbuf, gain_b], writes=[xsb])
            pb = n % 2

            def tr(e, xs=xs, pb=pb):
                ins = None
                for c in range(8):
                    ins = e.transpose(out=PB[pb][:, c * 128:(c + 1) * 128], in_=xs[:, c * 128:(c + 1) * 128],
                                      identity=ident)
                return ins
            P.op("pe", tr, reads=[xsb, constb], writes=[pbb[pb]])
            dst = dstT[:, :, n * 128:(n + 1) * 128]
            P.op("act", lambda e, dst=dst, pb=pb: e.activation(
                out=dst, in_=PB[pb].rearrange("p (c t) -> p c t", c=8), func=AF.Copy),
                reads=[pbb[pb]], writes=[dst_bufs[n]])

    def postnorm_tile(y_ap, y_bufs, gain_ap, gain_b, t, tmp_ap, tmp_b):
        st, stbuf = stat()
        for hf in range(2):
            hs = slice(hf * 512, (hf + 1) * 512)
            P.op("act", lambda e, hf=hf, hs=hs: e.activation(out=junk[:, hs], in_=y_ap[:, hs], func=AF.Square,
                                                              accum_out=st[:, 3 + hf:4 + hf]),
                 reads=[y_bufs[hf % len(y_bufs)]], writes=[stbuf])
        P.op("pool", lambda e: e.tensor_tensor(out=st[:, 0:1], in0=st[:, 3:4], in1=st[:, 4:5], op=ALU.add),
             reads=[stbuf], writes=[stbuf])
        rstd_from_ss(st[:, 0:1], stbuf, D, st[:, 2:3], stbuf, st[:, 1:2])
        for hf in range(2):
            hs = slice(hf * 512, (hf + 1) * 512)
            P.op("dve", lambda e, hs=hs: e.scalar_tensor_tensor(out=tmp_ap[:, hs], in0=y_ap[:, hs], scalar=st[:, 2:3],
                                                                in1=gain_ap[:, hs], op0=ALU.mult, op1=ALU.mult),
                 reads=[y_bufs[hf % len(y_bufs)], stbuf, gain_b], writes=[tmp_b])
        P.op("pool", lambda e: e.tensor_tensor(out=hview[:, t, :], in0=hview[:, t, :], in1=tmp_ap, op=ALU.add),
             reads=[tmp_b, hb[t]], writes=[hb[t]])

    def out_proj(srcT, src_bufs_fn, nk, w_ap, wbuf, gain_ap, gain_b, tmp_view, tmp_bufs):
        for t in range(NT):
            yb = t % 2
            ybufs = [pfb[2 * yb], pfb[2 * yb + 1]]
            y_ap = PS[:, yb * 1024:(yb + 1) * 1024]

            def mm(e, t=t, yb=yb):
                ins = None
                for half in range(2):
                    for k in range(nk):
                        ins = e.matmul(PF[2 * yb + half], lhsT=srcT[:, k, t * 128:(t + 1) * 128],
                                       rhs=w_ap[:, k, half * 512:(half + 1) * 512],
                                       start=(k == 0), stop=(k == nk - 1))
                return ins
            P.op("pe", mm, reads=src_bufs_fn(t) + [wbuf], writes=ybufs)
            postnorm_tile(y_ap, ybufs, gain_ap, gain_b, t, tmp_view[:, t % 2, :], tmp_bufs[t % 2])

    def mlp(layer, final, s):
        wu_sb = [view(RO, 32768 + j * 8192, [8, 512], BF16) for j in range(2)]
        wd_sb = [view(RO, 49152 + j * 8192, [4, 1024], BF16) for j in range(2)]
        wub = [Buf(), Buf()]
        wdb = [Buf(), Buf()]
        ysb = view(RO, 0, [8, 1024], F32)
        yb_ = [Buf() for _ in range(8)]
        xn2Ts = [RX[:, hh * 8192:(hh + 1) * 8192].rearrange("p (c t) -> p c t", c=8) for hh in range(2)]
        xn2bs = [[Buf() for _ in range(8)] for _ in range(2)]
        xs_view = view(RS, 0, [2, 1024], BF16)
        xs_bufs = [Buf(), Buf()]
        r_v = view(RS, 4096, [2, 512], F32)
        r_b = [Buf(), Buf()]
        uT = [view(RS, 16384 + g * 4096, [4, 512], BF16) for g in range(2)]
        uTb = [[Buf() for _ in range(4)] for _ in range(2)]
        tmp_view = view(RS, 8192, [2, 1024], F32)
        tmp_bufs = [Buf(), Buf()]
        yb_ = [[Buf(), Buf()] for _ in range(8)]
        gpre, gpreb = load_gain(layer * 4 + 2)

        def pre(hh):
            prenorm([hh * 8 + i for i in range(8)], lambda t: (hview[:, t, :], hb[t]), gpre, gpreb,
                    xn2Ts[hh], xn2bs[hh], xs_view, xs_bufs)
        pre(0)
        for half in range(2):
            xn2T, xn2b = xn2Ts[half], xn2bs[half]
            uc = 0
            yc = 0
            ugc = 0
            rc = 0
            for j in range(8):
                sl = j % 2
                P.dma("sp", "wu%d" % sl, lambda e, sl=sl, j=j: e.dma_start(
                    out=wu_sb[sl], in_=wu_s[layer][j].rearrange("(k p) c -> p k c", p=128)),
                    reads=[cvb["wu%d_%d" % (layer, j)]], writes=[wub[sl]])
                P.dma("sp", "wd%d" % sl, lambda e, sl=sl, j=j: e.dma_start(
                    out=wd_sb[sl], in_=wd_s[layer][j].rearrange("(c p) n -> p c n", p=128)),
                    reads=[cvb["wd%d_%d" % (layer, j)]], writes=[wdb[sl]])
                if half == 0 and j == 4:
                    pre(1)
                for tg in range(2):
                    ug = ugc % 2
                    ugc += 1
                    for ffc in range(4):
                        ub = uc % 3
                        uc += 1
                        rb = rc % 2
                        rc += 1

                        def mmu(e, sl=sl, ffc=ffc, tg=tg, ub=ub, xn2T=xn2T):
                            ins = None
                            for k in range(8):
                                ins = e.matmul(PF[ub], lhsT=wu_sb[sl][:, k, ffc * 128:(ffc + 1) * 128],
                                               rhs=xn2T[:, k, tg * 512:(tg + 1) * 512],
                                               start=(k == 0), stop=(k == 7))
                            return ins
                        P.op("pe", mmu, reads=[wub[sl]] + xn2b[4 * tg:4 * tg + 4], writes=[pfb[ub]])
                        P.op("act", lambda e, ub=ub, rb=rb: e.activation(out=r_v[:, rb, :], in_=PF[ub], func=AF.Relu),
                             reads=[pfb[ub]], writes=[r_b[rb]])
                        P.op("dve", lambda e, rb=rb, ug=ug, ffc=ffc: e.tensor_tensor(
                            out=uT[ug][:, ffc, :], in0=r_v[:, rb, :], in1=r_v[:, rb, :], op=ALU.mult),
                            reads=[r_b[rb]], writes=[uTb[ug][ffc]])
                    for tt in range(4):
                        ti = 4 * tg + tt
                        for h2 in range(2):
                            yb = 3 + yc % 3
                            yc += 1
                            hs = slice(h2 * 512, (h2 + 1) * 512)

                            def mmd(e, sl=sl, ug=ug, tt=tt, hs=hs, yb=yb):
                                ins = None
                                for ffc in range(4):
                                    ins = e.matmul(PF[yb], lhsT=uT[ug][:, ffc, tt * 128:(tt + 1) * 128],
                                                   rhs=wd_sb[sl][:, ffc, hs], start=(ffc == 0), stop=(ffc == 3))
                                return ins
                            P.op("pe", mmd, reads=uTb[ug] + [wdb[sl]], writes=[pfb[yb]])
                            if j == 0:
                                P.op("dve", lambda e, ti=ti, yb=yb, hs=hs: e.tensor_copy(out=ysb[:, ti, hs], in_=PF[yb]),
                                     reads=[pfb[yb]], writes=[yb_[ti][h2]])
                            else:
                                P.op("dve", lambda e, ti=ti, yb=yb, hs=hs: e.tensor_tensor(
                                    out=ysb[:, ti, hs], in0=PF[yb], in1=ysb[:, ti, hs], op=ALU.add),
                                    reads=[pfb[yb], yb_[ti][h2]], writes=[yb_[ti][h2]])
            gpost, gpostb = load_gain(layer * 4 + 3)
            for i in range(8):
                t = half * 8 + i
                postnorm_tile(ysb[:, i, :], yb_[i], gpost, gpostb, t, tmp_view[:, i % 2, :], tmp_bufs[i % 2])
                if final:
                    P.dma("sp", "outd%d" % t, lambda e, t=t: e.dma_start(out=out_d[s, t * 128:(t + 1) * 128, :],
                                                                  in_=hview[:, t, :]), reads=[hb[t]])

    CD = [float((1.0 - 2.0 ** (-5.0 - h)) ** 128) for h in range(4)]

    def layer0(s, sub=3, nheads=4, ngroups=4):
        xin = view(RS, 0, [2, 1024], F32)
        xinb = [Buf(), Buf()]
        xs_view = view(RS, 8192, [2, 1024], BF16)
        xs_bufs = [Buf(), Buf()]
        gpre, gpreb = load_gain(0)

        def src_fn(t):
            sl = t % 2
            P.dma("sp", "xin%d" % sl, lambda e, sl=sl, t=t: e.dma_start(out=xin[:, sl, :],
                                                                      in_=x_d[s, t * 128:(t + 1) * 128, :]),
                  writes=[xinb[sl]])
            return xin[:, sl, :], xinb[sl]
        prenorm(list(range(NT)), src_fn, gpre, gpreb, xnT, xnb, xs_view, xs_bufs)
        P.barrier()
        if sub == 1:
            for t in range(NT):
                P.dma("sp", "xh%d" % t, lambda e, t=t: e.dma_start(out=hview[:, t, :], in_=x_d[s, t * 128:(t + 1) * 128, :]),
                      writes=[hb[t]])
            return
        wh = [view(RH, j * 24576, [8, 1536], BF16) for j in range(2)]
        whb = [Buf(), Buf()]
        cs = view(RH, 49152, [2, S], F32)
        csb = Buf()
        P.dma("sp", "cs", lambda e: e.dma_start(out=cs[:, 0, :], in_=cs0_d[0]), writes=[csb])
        P.dma("sp", "cs", lambda e: e.dma_start(out=cs[:, 1, :], in_=cs0_d[1]), writes=[csb])
        ogT = RO[:, :].rearrange("p (c t) -> p c t", c=16)
        ogb = [Buf() for _ in range(NT)]
        S32 = view(RS, 0, [2, 512], F32)
        Sbf = view(RS, 4096, [2, 512], BF16)
        s32b, sbfb = Buf(), Buf()
        qT = view(RS, 6144, [2, 512], BF16)
        kT = view(RS, 8192, [2, 512], BF16)
        qsT = view(RS, 10240, [2, 512], BF16)
        qTb, kTb, qsTb = Buf(), Buf(), Buf()
        tt_ = [view(RS, 12288 + i * 2048, [512], F32) for i in range(4)]
        ttb = [Buf() for _ in range(4)]
        gb16 = gbuf[:, 0, :].bitcast(BF16)
        v_bfs = [view(RS, 20480, [512], BF16), gb16[:, 0:512]]
        sgs = [view(RS, 21504, [512], BF16), gb16[:, 512:1024]]
        vbs, sgbs = [Buf(), Buf()], [Buf(), Buf()]
        sT_bf = view(RS, 22528, [128], BF16)
        kd_bf = view(RS, 22784, [256], BF16)
        og_bf = view(RS, 23296, [512], BF16)
        sTb, kdb, ogbf = Buf(), Buf(), Buf()
        def proj_vg(ci, w, sl):
            tk = slice(ci * 128, (ci + 1) * 128)
            v_bf, sg, vb, sgb = v_bfs[ci % 2], sgs[ci % 2], vbs[ci % 2], sgbs[ci % 2]

            def mmv(e):
                ins = None
                for k in range(8):
                    ins = e.matmul(PF[0], lhsT=xnT[:, k, tk], rhs=w[:, k, 512:1024], start=(k == 0), stop=(k == 7))
                return ins
            P.op("pe", mmv, reads=[whb[sl], xnb[ci]], writes=[pfb[0]])
            P.op("act", lambda e: e.activation(out=v_bf, in_=PF[0], func=AF.Copy), reads=[pfb[0]], writes=[vb])

            def mmg(e):
                ins = None
                for k in range(8):
                    ins = e.matmul(PF[1], lhsT=xnT[:, k, tk], rhs=w[:, k, 1024:1536], start=(k == 0), stop=(k == 7))
                return ins
            P.op("pe", mmg, reads=[whb[sl], xnb[ci]], writes=[pfb[1]])
            P.op("act", lambda e: e.activation(out=sg, in_=PF[1], func=AF.Silu), reads=[pfb[1]], writes=[sgb])

        for h in range(nheads):
            sl = h % 2
            P.dma("sp", "wh%d" % sl, lambda e, sl=sl, h=h: e.dma_start(
                out=wh[sl], in_=w0h_s[h].rearrange("(k p) c -> p k c", p=128)),
                reads=[cvb["w0h_%d" % h]], writes=[whb[sl]])
            w = wh[sl]
            for G in range(ngroups):
                gs = slice(G * 512, (G + 1) * 512)
                banks = [0, 1, 4, 5]
                for fc in range(4):
                    def mmqk(e, fc=fc, w=w, gs=gs):
                        ins = None
                        for k in range(8):
                            ins = e.matmul(PF[banks[fc]], lhsT=w[:, k, fc * 128:(fc + 1) * 128],
                                           rhs=xnT[:, k, gs], start=(k == 0), stop=(k == 7))
                        return ins
                    P.op("pe", mmqk, reads=[whb[sl]] + xnb[G * 4:(G + 1) * 4], writes=[pfb[banks[fc]]])
                cosg = cs[:, 0, gs]
                sing = cs[:, 1, gs]
                for which, (b1, b2, dstT, dstb) in enumerate(((0, 1, qT, qTb), (4, 5, kT, kTb))):
                    x1, x2 = PF[b1], PF[b2]
                    P.op("dve", lambda e, x1=x1, cosg=cosg: e.tensor_tensor(out=tt_[0], in0=x1, in1=cosg, op=ALU.mult),
                         reads=[pfb[b1], csb], writes=[ttb[0]])
                    P.op("dve", lambda e, x2=x2, sing=sing: e.tensor_tensor(out=tt_[1], in0=x2, in1=sing, op=ALU.mult),
                         reads=[pfb[b2], csb], writes=[ttb[1]])
                    P.op("pool", lambda e, dstT=dstT: e.tensor_tensor(out=dstT[:, 0, :], in0=tt_[0], in1=tt_[1],
                                                                      op=ALU.subtract),
                         reads=[ttb[0], ttb[1]], writes=[dstb])
                    P.op("dve", lambda e, x1=x1, sing=sing: e.tensor_tensor(out=tt_[2], in0=x1, in1=sing, op=ALU.mult),
                         reads=[pfb[b1], csb], writes=[ttb[2]])
                    P.op("dve", lambda e, x2=x2, cosg=cosg: e.tensor_tensor(out=tt_[3], in0=x2, in1=cosg, op=ALU.mult),
                         reads=[pfb[b2], csb], writes=[ttb[3]])
                    P.op("pool", lambda e, dstT=dstT: e.tensor_tensor(out=dstT[:, 1, :], in0=tt_[2], in1=tt_[3],
                                                                      op=ALU.add),
                         reads=[ttb[2], ttb[3]], writes=[dstb])
                P.op("pool", lambda e, h=h: e.tensor_tensor(
                    out=qsT.rearrange("p a (c i) -> p (a c) i", i=128),
                    in0=qT.rearrange("p a (c i) -> p (a c) i", i=128),
                    in1=qdec[:, h:h + 1, :].to_broadcast([128, 8, 128]), op=ALU.mult),
                    reads=[qTb, constb], writes=[qsTb])
                for c in range(4):
                    ci = G * 4 + c
                    tk = slice(ci * 128, (ci + 1) * 128)
                    tg = slice(c * 128, (c + 1) * 128)

                    v_bf, sg, vb, sgb = v_bfs[ci % 2], sgs[ci % 2], vbs[ci % 2], sgbs[ci % 2]
                    if ci == 0:
                        proj_vg(0, w, sl)

                    def mms(e, tg=tg):
                        ins = None
                        for dc in range(2):
                            ins = e.matmul(PF[2][:, 0:128], lhsT=kT[:, dc, tg], rhs=qT[:, dc, tg],
                                           start=(dc == 0), stop=(dc == 1))
                        return ins
                    P.op("pe", mms, reads=[qTb, kTb], writes=[pfb[2]])
                    P.op("dve", lambda e, h=h: e.tensor_tensor(out=sT_bf, in0=PF[2][:, 0:128], in1=maskT[:, h, :],
                                                               op=ALU.mult),
                         reads=[pfb[2], constb], writes=[sTb])

                    def trk(e, tg=tg):
                        ins = None
                        for dc in range(2):
                            ins = e.transpose(out=PB[0][:, dc * 128:(dc + 1) * 128], in_=kT[:, dc, tg], identity=ident)
                        return ins
                    P.op("pe", trk, reads=[kTb, constb], writes=[pbb[0]])
                    P.op("dve", lambda e, h=h: e.tensor_scalar(out=kd_bf, in0=PB[0][:, 0:256], scalar1=kdec[:, h:h + 1],
                                                               scalar2=None, op0=ALU.mult),
                         reads=[pbb[0], constb], writes=[kdb])

                    def mmo(e, ci=ci, tg=tg, v_bf=v_bf):
                        ins = e.matmul(PF[3], lhsT=sT_bf, rhs=v_bf, start=True, stop=(ci == 0))
                        if ci > 0:
                            for dc in range(2):
                                ins = e.matmul(PF[3], lhsT=qsT[:, dc, tg], rhs=Sbf[:, dc, :], start=False, stop=(dc == 1))
                        return ins
                    P.op("pe", mmo, reads=[sTb, vb, qsTb, sbfb], writes=[pfb[3]])
                    if ci < 15:
                        for dc in range(2):
                            P.op("pe", lambda e, dc=dc, v_bf=v_bf: e.matmul(PF[4 + dc], lhsT=kd_bf[:, dc * 128:(dc + 1) * 128],
                                                                             rhs=v_bf, start=True, stop=True),
                                 reads=[kdb, vb], writes=[pfb[4 + dc]])
                    st, stbuf = stat()
                    P.op("act", lambda e, st=st: e.activation(out=junk[:, 0:512], in_=PF[3], func=AF.Square,
                                                               accum_out=st[:, 0:1]),
                         reads=[pfb[3]], writes=[stbuf])
                    rstd_from_ss(st[:, 0:1], stbuf, 512, st[:, 2:3], stbuf, st[:, 1:2])
                    if ci < 15:
                        proj_vg(ci + 1, w, sl)
                    P.op("dve", lambda e, st=st, sg=sg: e.scalar_tensor_tensor(out=og_bf, in0=PF[3], scalar=st[:, 2:3], in1=sg,
                                                                               op0=ALU.mult, op1=ALU.mult),
                         reads=[pfb[3], stbuf, sgb], writes=[ogbf])

                    def tro(e):
                        ins = None
                        for ec in range(4):
                            ins = e.transpose(out=PB[1][:, ec * 128:(ec + 1) * 128], in_=og_bf[:, ec * 128:(ec + 1) * 128],
                                              identity=ident)
                        return ins
                    P.op("pe", tro, reads=[ogbf, constb], writes=[pbb[1]])
                    P.op("act", lambda e, h=h, tk=tk: e.activation(
                        out=ogT[:, h * 4:(h + 1) * 4, tk], in_=PB[1][:, 0:512].rearrange("p (c t) -> p c t", c=4),
                        func=AF.Copy), reads=[pbb[1]], writes=[ogb[ci]])
                    if ci < 15:
                        for dc in range(2):
                            if ci == 0:
                                P.op("dve", lambda e, dc=dc: e.tensor_copy(out=S32[:, dc, :], in_=PF[4 + dc]),
                                     reads=[pfb[4 + dc]], writes=[s32b])
                            else:
                                P.op("dve", lambda e, dc=dc, h=h: e.scalar_tensor_tensor(
                                    out=S32[:, dc, :], in0=S32[:, dc, :], scalar=CD[h], in1=PF[4 + dc],
                                    op0=ALU.mult, op1=ALU.add), reads=[pfb[4 + dc], s32b], writes=[s32b])
                        P.op("pool", lambda e: e.tensor_copy(out=Sbf, in_=S32), reads=[s32b], writes=[sbfb])
        P.barrier()
        if sub == 2:
            for t in range(NT):
                P.dma("sp", "xh%d" % t, lambda e, t=t: e.dma_start(out=hview[:, t, :], in_=x_d[s, t * 128:(t + 1) * 128, :]),
                      writes=[hb[t]])
            return
        wo = RX[:, :].rearrange("p (k c) -> p k c", k=16)
        wob = Buf()
        P.dma("sp", "wo", lambda e: e.dma_start(out=wo, in_=wo0_s.rearrange("(k p) c -> p k c", p=128)),
              reads=[cvb["wo0"]], writes=[wob])
        for t in range(NT):
            P.dma("sp", "xh%d" % t, lambda e, t=t: e.dma_start(out=hview[:, t, :], in_=x_d[s, t * 128:(t + 1) * 128, :]),
                  writes=[hb[t]])
        gpost, gpostb = load_gain(1)
        tmp_view = view(RS, 0, [2, 1024], F32)
        tmp_bufs = [Buf(), Buf()]
        out_proj(ogT, lambda t: [ogb[t]], 16, wo, wob, gpost, gpostb, tmp_view, tmp_bufs)
        P.barrier()

    SC = float(128 ** -0.5)

    def layer1(s):
        import os
        sub1 = int(os.environ.get('L1SUB', 9))
        nh1 = int(os.environ.get('L1NH', 8))
        nt1 = int(os.environ.get('L1NT', 16))
        l1p = int(os.environ.get('L1P', 63))
        xs_view = view(RS, 0, [2, 1024], BF16)
        xs_bufs = [Buf(), Buf()]
        gpre, gpreb = load_gain(4)
        prenorm(list(range(NT)), lambda t: (hview[:, t, :], hb[t]), gpre, gpreb, xnT, xnb, xs_view, xs_bufs)
        P.barrier()
        if sub1 == 1:
            return
        oT = RO[:, 0:16384].rearrange("p (c t) -> p c t", c=8)
        oTb = [Buf() for _ in range(NT)]
        cs = view(RO, 32768, [2, S], F32)
        csb = Buf()
        P.dma("sp", "cs", lambda e: e.dma_start(out=cs[:, 0, :], in_=cs1_d[0]), writes=[csb])
        P.dma("sp", "cs", lambda e: e.dma_start(out=cs[:, 1, :], in_=cs1_d[1]), writes=[csb])
        wh = [view(RO, 49152 + j * 6144, [8, 384], BF16) for j in range(2)]
        whb = [Buf(), Buf()]
        t1 = view(RO, 61440, [512], F32)
        t2 = view(RO, 63488, [512], F32)
        if int(os.environ.get('E2', 0)) == 1:
            t1 = gbuf[:, 0, 0:512]
            t2 = gbuf[:, 0, 512:1024]
        if int(os.environ.get('E2', 0)) == 2:
            t1 = view(RO, 55296, [512], F32)
            t2 = view(RO, 57344, [512], F32)
        if int(os.environ.get('E2', 0)) == 3:
            t1 = view(RO, 59392, [512], F32)
            t2 = view(RO, 61440, [512], F32)
        t1b, t2b = Buf(), Buf()
        qT = view(RS, 0, [S], BF16)
        kT = view(RS, 4096, [S], BF16)
        vh = view(RS, 8192, [16, 128], BF16)
        p_bf = view(RS, 12288, [S], BF16)
        pT = view(RS, 16384, [16, 128], BF16)
        sown = view(RS, 20480, [256], F32)
        o_bf = view(RS, 21504, [128], BF16)
        kmT = view(RS, 21760, [8], BF16)
        ksum = view(RS, 21776, [8], F32)
        qTb = [Buf() for _ in range(4)]
        kTb = [Buf() for _ in range(4)]
        vhb, pTb, obfb, kmb = Buf(), Buf(), Buf(), Buf()
        pbufs = [p_bf, gbuf[:, 1, :].bitcast(BF16)]
        pbs = [Buf(), Buf()]
        sowns = [sown, gbuf[:, 0, 0:256]]
        sownbs = [Buf(), Buf()]
        unit_ctr = [0]
        for h in range(nh1):
            sl = h % 2
            P.dma("sp", "wh%d" % sl, lambda e, sl=sl, h=h: e.dma_start(
                out=wh[sl], in_=w1h_s[h].rearrange("(k p) c -> p k c", p=128)),
                reads=[cvb["w1h"]], writes=[whb[sl]])
            w = wh[sl]
            for which, (dst, dstb) in enumerate(((qT, qTb), (kT, kTb))):
                for G in range(4):
                    gs = slice(G * 512, (G + 1) * 512)

                    def mmp(e, w=w, gs=gs, which=which, G=G):
                        ins = None
                        for k in range(8):
                            ins = e.matmul(PF[G], lhsT=w[:, k, which * 128:(which + 1) * 128], rhs=xnT[:, k, gs],
                                           start=(k == 0), stop=(k == 7))
                        return ins
                    P.op("pe", mmp, reads=[whb[sl]] + xnb[G * 4:(G + 1) * 4], writes=[pfb[G]])
                for G in range(4):
                    gs = slice(G * 512, (G + 1) * 512)
                    P.op("act", lambda e, dst=dst, gs=gs, G=G: e.activation(out=dst[:, gs], in_=PF[G], func=AF.Copy),
                         reads=[pfb[G]], writes=[dstb[G]])
                for G in range(4):
                    gs = slice(G * 512, (G + 1) * 512)
                    P.op("pe", lambda e, dst=dst, gs=gs: e.matmul(PF[4], lhsT=Rmat, rhs=dst[:, gs],
                                                                   start=True, stop=True),
                         reads=[dstb[G], constb], writes=[pfb[4]])
                    P.op("dve", lambda e, G=G, gs=gs: e.tensor_tensor(out=t1, in0=PF[G], in1=cs[:, 0, gs],
                                                                      op=ALU.mult),
                         reads=[pfb[G], csb, dstb[G]], writes=[t1b])
                    P.op("dve", lambda e, gs=gs: e.tensor_tensor(out=t2, in0=PF[4], in1=cs[:, 1, gs],
                                                                 op=ALU.mult),
                         reads=[pfb[4], csb], writes=[t2b])
                    P.op("pool", lambda e, dst=dst, gs=gs: e.tensor_tensor(out=dst[:, gs], in0=t1, in1=t2, op=ALU.add),
                         reads=[t1b, t2b], writes=[dstb[G]])
            for t4 in range(4 if (l1p & 4) else 0):
                def mmv(e, w=w, t4=t4):
                    ins = None
                    for tt in range(4):
                        t = t4 * 4 + tt
                        for k in range(8):
                            ins = e.matmul(PF[5][:, tt * 128:(tt + 1) * 128], lhsT=xnT[:, k, t * 128:(t + 1) * 128],
                                           rhs=w[:, k, 256:384], start=(k == 0), stop=(k == 7))
                    return ins
                P.op("pe", mmv, reads=[whb[sl]] + xnb[t4 * 4:(t4 + 1) * 4], writes=[pfb[5]])
                P.op("act", lambda e, t4=t4: e.activation(out=vh[:, t4 * 4:(t4 + 1) * 4, :],
                                                          in_=PF[5].rearrange("p (a b) -> p a b", a=4), func=AF.Copy),
                     reads=[pfb[5]], writes=[vhb])
            if l1p & 8:
                P.op("dve", lambda e: e.tensor_reduce(out=ksum, in_=kT.rearrange("p (n j) -> p n j", n=8), axis=AX.X,
                                                      op=ALU.add), reads=kTb, writes=[kmb])
                P.op("dve", lambda e: e.tensor_copy(out=kmT, in_=ksum), reads=[kmb], writes=[kmb])
            def unitA(t, par, ctx):
                b = t // 2
                odd = t % 2
                tq = slice(t * 128, (t + 1) * 128)
                nown = 128 * (1 + odd)
                nkeys = b * 256 + nown
                nkt = nkeys // 128
                nbank = (nkeys + 511) // 512
                base = 2 * par if nbank <= 2 else 0
                bk = lambda j: base + j
                p_cur, pcb = pbufs[par], pbs[par]
                so, sob = sowns[par], sownbs[par]
                qbuf = [qTb[t // 4]]
                for j in range(nbank):
                    wdt = min(512, nkeys - j * 512)
                    P.op("pe", lambda e, j=j, wdt=wdt, bj=bk(j): e.matmul(PF[bj][:, 0:wdt], lhsT=qT[:, tq],
                                                                          rhs=kT[:, j * 512:j * 512 + wdt],
                                                                          start=True, stop=True),
                         reads=qbuf + kTb, writes=[pfb[bk(j)]])
                st, stbuf = stat()
                st2, stbuf2 = stat()
                st3, stbuf3 = stat()
                if b >= 4:
                    P.op("pe", lambda e: e.matmul(PF[4][:, 0:8], lhsT=qT[:, tq], rhs=kmT, start=True, stop=True),
                         reads=qbuf + [kmb], writes=[pfb[4]])
                    P.op("dve", lambda e: e.tensor_tensor(out=st, in0=PF[4][:, 0:8], in1=pastm[:, b, :], op=ALU.add),
                         reads=[pfb[4], constb], writes=[stbuf])
                    stm, stmb = stat()
                    P.op("dve", lambda e: e.max(out=stm, in_=st), reads=[stbuf], writes=[stmb])
                    P.op("dve", lambda e: e.tensor_scalar(out=st, in0=st, scalar1=stm[:, 2:3], scalar2=None, op0=ALU.is_ge),
                         reads=[stbuf, stmb], writes=[stbuf])
                    P.op("dve", lambda e: e.tensor_scalar(out=st, in0=st, scalar1=-1.0, scalar2=BIG, op0=ALU.add,
                                                          op1=ALU.mult), reads=[stbuf], writes=[stbuf])
                    selb = st
                    selbuf = stbuf
                else:
                    selb = zeros8
                    selbuf = constb
                jo, oo = divmod(b * 256, 512)
                own_ps = PF[bk(jo)][:, oo:oo + nown]
                P.op("dve", lambda e: e.tensor_tensor(out=so[:, 0:nown], in0=own_ps, in1=causal[:, odd, 0:nown], op=ALU.add),
                     reads=[pfb[bk(jo)], constb], writes=[sob])
                P.op("dve", lambda e: e.tensor_reduce(out=st2[:, b:b + 1], in_=so[:, 0:nown], axis=AX.X, op=ALU.max),
                     reads=[sob], writes=[stbuf2])
                for j in range((b * 256 + 511) // 512):
                    nb_here = min(2, b - 2 * j)
                    P.op("dve", lambda e, j=j, nb_here=nb_here, bj=bk(j): e.tensor_reduce(
                        out=st2[:, 2 * j:2 * j + nb_here],
                        in_=PF[bj][:, 0:nb_here * 256].rearrange("p (n k) -> p n k", n=nb_here), axis=AX.X, op=ALU.max),
                        reads=[pfb[bk(j)]], writes=[stbuf2])
                yield
                if b > 0:
                    P.op("dve", lambda e: e.tensor_tensor(out=st2[:, 0:b], in0=st2[:, 0:b], in1=selb[:, 0:b], op=ALU.add),
                         reads=[stbuf2, selbuf], writes=[stbuf2])
                stn, stnb = stat()
                P.op("dve", lambda e: e.tensor_reduce(out=stn[:, 0:1], in_=st2[:, 0:b + 1], axis=AX.X, op=ALU.max),
                     reads=[stbuf2], writes=[stnb])
                P.op("dve", lambda e: e.tensor_scalar(out=stn[:, 1:2], in0=stn[:, 0:1], scalar1=-SC, scalar2=None,
                                                      op0=ALU.mult), reads=[stnb], writes=[stnb])
                if b > 0:
                    P.op("dve", lambda e: e.tensor_scalar(out=st2[:, 0:b], in0=selb[:, 0:b], scalar1=stn[:, 1:2],
                                                          scalar2=None, op0=ALU.add),
                         reads=[selbuf, stnb, stbuf2], writes=[stbuf2])
                yield
                for n in range(b):
                    j, o = divmod(n * 256, 512)
                    P.op("act", lambda e, n=n, o=o, bj=bk(j): e.activation(
                        out=p_cur[:, n * 256:(n + 1) * 256], in_=PF[bj][:, o:o + 256], func=AF.Exp,
                        bias=st2[:, n:n + 1], scale=SC, accum_out=st3[:, n:n + 1]),
                        reads=[pfb[bk(j)], stbuf2], writes=[pcb, stbuf3])
                P.op("act", lambda e: e.activation(
                    out=p_cur[:, b * 256:b * 256 + nown], in_=so[:, 0:nown], func=AF.Exp,
                    bias=stn[:, 1:2], scale=SC, accum_out=st3[:, b:b + 1]),
                    reads=[sob, stnb], writes=[pcb, stbuf3])
                P.op("dve", lambda e: e.tensor_reduce(out=stn[:, 2:3], in_=st3[:, 0:b + 1], axis=AX.X, op=ALU.add),
                     reads=[stbuf3, stnb], writes=[stnb])
                P.op("dve", lambda e: e.reciprocal(out=stn[:, 3:4], in_=stn[:, 2:3]), reads=[stnb], writes=[stnb])
                ctx.update(t=t, tq=tq, nkt=nkt, p_cur=p_cur, pcb=pcb, stn=stn, stnb=stnb)

            def unitB(c):
                t, tq, nkt, p_cur, pcb, stn, stnb = c["t"], c["tq"], c["nkt"], c["p_cur"], c["pcb"], c["stn"], c["stnb"]
                for pbk in range((nkt + 7) // 8):
                    n_here = min(8, nkt - pbk * 8)

                    def trp(e, pbk=pbk, n_here=n_here):
                        ins = None
                        for i in range(n_here):
                            kt = pbk * 8 + i
                            ins = e.transpose(out=PB[pbk][:, i * 128:(i + 1) * 128], in_=p_cur[:, kt * 128:(kt + 1) * 128],
                                              identity=ident)
                        return ins
                    P.op("pe", trp, reads=[pcb, constb], writes=[pbb[pbk]])
                    src3 = PB[pbk][:, 0:n_here * 128].rearrange("p (a b) -> p a b", a=n_here)
                    dst3 = pT[:, pbk * 8:pbk * 8 + n_here, :]
                    if pbk == 0:
                        P.op("act", lambda e, src3=src3, dst3=dst3: e.activation(out=dst3, in_=src3, func=AF.Copy),
                             reads=[pbb[pbk]], writes=[pTb])
                    else:
                        P.op("dve", lambda e, src3=src3, dst3=dst3: e.tensor_copy(out=dst3, in_=src3),
                             reads=[pbb[pbk]], writes=[pTb])

                yield
                def mmpv(e):
                    ins = None
                    for kt in range(nkt):
                        ins = e.matmul(PF[5][:, 0:128], lhsT=pT[:, kt, :], rhs=vh[:, kt, :], start=(kt == 0),
                                       stop=(kt == nkt - 1))
                    return ins
                P.op("pe", mmpv, reads=[pTb, vhb], writes=[pfb[5]])
                P.op("dve", lambda e: e.tensor_scalar(out=o_bf, in0=PF[5][:, 0:128], scalar1=stn[:, 3:4], scalar2=None,
                                                      op0=ALU.mult), reads=[pfb[5], stnb], writes=[obfb])
                P.op("pe", lambda e: e.transpose(out=PB[1][:, 896:1024], in_=o_bf, identity=ident),
                     reads=[obfb, constb], writes=[pbb[1]])
                P.op("act", lambda e, h=h: e.activation(out=oT[:, h, tq], in_=PB[1][:, 896:1024], func=AF.Copy),
                     reads=[pbb[1]], writes=[oTb[t]])

            def drain(g):
                if g is not None:
                    for _ in g:
                        pass

            def step(g):
                if g is not None:
                    next(g, None)

            prevc = None
            for t in range(nt1 if sub1 >= 3 else 0):
                ctx = {}
                ga = unitA(t, unit_ctr[0] % 2, ctx)
                unit_ctr[0] += 1
                gb_ = unitB(prevc) if prevc is not None else None
                step(ga)
                step(gb_)
                step(ga)
                drain(gb_)
                drain(ga)
                prevc = ctx
            if prevc is not None:
                drain(unitB(prevc))
        P.barrier()
        if sub1 <= 3:
            return
        wo = RX[:, 0:8192].rearrange("p (k c) -> p k c", k=8)
        wob = Buf()
        P.dma("sp", "wo", lambda e: e.dma_start(out=wo, in_=wo1_s.rearrange("(k p) c -> p k c", p=128)),
              reads=[cvb["wo1"]], writes=[wob])
        gpost, gpostb = load_gain(5)
        tmp_view = view(RS, 0, [2, 1024], F32)
        tmp_bufs = [Buf(), Buf()]
        out_proj(oT, lambda t: [oTb[t]], 8, wo, wob, gpost, gpostb, tmp_view, tmp_bufs)
        P.barrier()

    P.dma("sp", "const", lambda e: e.dma_start(out=cmask[:, :], in_=cmask_d), writes=[constb])
    P.dma("pool", "constb", lambda e: e.dma_start(out=cbf[:, :], in_=cbf_d), writes=[constb])
    P.barrier()
    for h in range(4):
        convert("w0h_%d" % h, w0h_d[h], w0h_s[h], 2)
    convert("wo0", wo0_d, wo0_s, 2)
    for j in range(8):
        convert("wu0_%d" % j, wu_d[0][j], wu_s[0][j], 1)
        convert("wd0_%d" % j, wd_d[0][j], wd_s[0][j], 1)
    if stop >= 3:
        convert("w1h", w1h_d, w1h_s, 3)
        convert("wo1", wo1_d, wo1_s, 1)
    if stop >= 4:
        for j in range(8):
            convert("wu1_%d" % j, wu_d[1][j], wu_s[1][j], 1)
            convert("wd1_%d" % j, wd_d[1][j], wd_s[1][j], 1)

    P.barrier(final=True)

    def dump(s):
        for t in range(NT):
            P.dma("sp", "outd%d" % t, lambda e, t=t: e.dma_start(out=out_d[s, t * 128:(t + 1) * 128, :], in_=hview[:, t, :]),
                  reads=[hb[t]])

    import os as _os
    only_l1 = int(_os.environ.get("ONLYL1", 0))
    for s in range(nseq):
        if only_l1:
            for t in range(NT):
                P.dma("sp", "xh%d" % t, lambda e, t=t, s=s: e.dma_start(out=hview[:, t, :], in_=x_d[s, t * 128:(t + 1) * 128, :]),
                      writes=[hb[t]])
            if int(_os.environ.get("E4", 0)):
                P.barrier(final=True)
            layer1(s)
            dump(s)
            P.barrier()
            continue
        layer0(s, sub, nheads, ngroups)
        if stop == 1:
            dump(s)
            P.barrier()
            continue
        mlp(0, False, s)
        P.barrier()
        if stop == 2:
            dump(s)
            P.barrier()
            continue
        layer1(s)
        if stop == 3:
            dump(s)
            P.barrier()
            continue
        mlp(1, True, s)
        P.barrier()
    P.barrier(final=True)
    P.emit(nc, es)
    es.close()
    return nc


def _consts():
    pos = np.arange(S, dtype=np.float32)
    freq = (1.0 / (np.float32(10000.0) ** np.linspace(0.0, 1.0, 128, dtype=np.float32))).astype(np.float32)
    ang0 = freq[:, None] * pos[None, :]
    cs0 = np.stack([np.cos(ang0), np.sin(ang0)]).astype(np.float32)
    inv = (np.float32(500000.0) ** (-np.arange(0, 32, 2, dtype=np.float32) / np.float32(32))).astype(np.float32)
    ang1 = inv[:, None] * pos[None, :]
    c1 = np.concatenate([np.cos(ang1), np.cos(ang1), np.ones((96, S), np.float32)], 0)
    s1 = np.concatenate([np.sin(ang1), np.sin(ang1), np.zeros((96, S), np.float32)], 0)
    cs1 = np.stack([c1, s1]).astype(np.float32)
    gam = 1.0 - 2.0 ** (-5.0 - np.arange(4, dtype=np.float64))
    idx = np.arange(128, dtype=np.float64)
    cm = np.zeros((128, 4 * 128 + 4 * 128 + 4 + 2 * 256 + 64 + 8 + 8), np.float32)
    for h in range(4):
        diff = idx[None, :] - idx[:, None]
        m = np.where(diff >= 0, gam[h] ** np.maximum(diff, 0.0), 0.0) / 16.0
        cm[:, h * 128:(h + 1) * 128] = m
        cm[:, 512 + h * 128:512 + (h + 1) * 128] = (gam[h] ** (idx + 1.0))[None, :]
        cm[:, 1024 + h] = gam[h] ** (127.0 - idx) / 16.0
    q = np.arange(128)
    key = np.arange(256)
    cm[:, 1028:1028 + 256] = np.where(key[None, :] <= q[:, None], 0.0, -BIG)
    cm[:, 1028 + 256:1028 + 512] = np.where(key[None, :] <= q[:, None] + 128, 0.0, -BIG)
    pm = np.zeros((8, 8), np.float32)
    for b in range(8):
        pm[b, b:] = -1e30
    cm[:, 1540:1604] = pm.reshape(1, 64)
    cm[:, 1604:1612] = -0.5
    cm[:, 1612:1620] = 0.0
    cb = np.zeros((128, 256), np.float32)
    cb[:, 0:128] = np.eye(128)
    for f in range(16):
        cb[f + 16, 128 + f] = -1.0
        cb[f, 128 + f + 16] = 1.0
    return cs0, cs1, cm, cb


def _prep(inputs):
    f = lambda n: np.asarray(inputs[n], dtype=np.float32)
    w_in_0 = f("w_in_0")
    blocks = []
    perm = np.concatenate([np.arange(0, 256, 2), np.arange(1, 256, 2)])
    for h in range(4):
        qh = w_in_0[:, h * 256:(h + 1) * 256][:, perm]
        kh = w_in_0[:, 1024 + h * 256:1024 + (h + 1) * 256][:, perm]
        vh = w_in_0[:, 2048 + h * 512:2048 + (h + 1) * 512]
        gh = w_in_0[:, 4096 + h * 512:4096 + (h + 1) * 512]
        blocks.append(np.concatenate([qh, kh, vh, gh], axis=1))
    w0h = np.ascontiguousarray(np.stack(blocks))
    w_in_1 = f("w_in_1")
    w1h = np.ascontiguousarray(np.stack([np.concatenate([w_in_1[:, h * 128:(h + 1) * 128],
                                                         w_in_1[:, 1024 + h * 128:1024 + (h + 1) * 128],
                                                         w_in_1[:, 2048 + h * 128:2048 + (h + 1) * 128]], axis=1)
                                         for h in range(8)]))
    wu = [np.ascontiguousarray(f(n).reshape(1024, 8, 512).transpose(1, 0, 2)) for n in ("w_up_0", "w_up_1")]
    wd = [np.ascontiguousarray(f(n).reshape(8, 512, 1024)) for n in ("w_down_0", "w_down_1")]
    names = ["ln_mix_pre_0", "ln_mix_post_0", "ln_mlp_pre_0", "ln_mlp_post_0",
             "ln_mix_pre_1", "ln_mix_post_1", "ln_mlp_pre_1", "ln_mlp_post_1"]
    g8 = np.ascontiguousarray(np.stack([np.broadcast_to(f(n)[None, :], (128, D)) for n in names]))
    cs0, cs1, cm, cb = _consts()
    return {"w0h": w0h, "wo0": f("w_out_0"), "wu0": wu[0], "wu1": wu[1], "wd0": wd[0], "wd1": wd[1],
            "w1h": w1h, "wo1": f("w_out_1"), "g8": g8, "cs0": cs0, "cs1": cs1, "cmask": cm, "cbf": cb}


def kernel(**inputs):
    x = np.asarray(inputs["x"], dtype=np.float32)
    shared = _prep(inputs)
    nc = build_program(nseq=2, stop=4)
    in_maps = []
    for c in range(NCORES):
        m = dict(shared)
        m["x"] = np.ascontiguousarray(x[2 * c:2 * c + 2])
        in_maps.append(m)
    res = run_bass_kernel_spmd(nc, in_maps, core_ids=list(range(NCORES)))
    return np.concatenate([np.asarray(r["out"]) for r in res.results], axis=0).astype(np.float32)
```

```python
import numpy as np
from contextlib import ExitStack
import concourse.bass as bass
import concourse.mybir as mybir
from concourse.bass_utils import run_bass_kernel_spmd

F32 = mybir.dt.float32
BF16 = mybir.dt.bfloat16
AF = mybir.ActivationFunctionType
ALU = mybir.AluOpType
AX = mybir.AxisListType

S = 2048
D = 1024
NT = 16
EPS = 1e-6
NCORES = 8
BIG = 30000.0
ENGS = ("pe", "act", "dve", "pool", "sp")


class Buf:
    __slots__ = ("w", "r")

    def __init__(self):
        self.w = None
        self.r = {}


class Prog:
    ROT = 8000

    def __init__(self):
        self.ops = {e: [] for e in ENGS}
        self.seen = {e: {} for e in ENGS}
        self.gen = {e: 0 for e in ENGS}
        self.cnt = {e: 0 for e in ENGS}
        self.semkeys = {}
        self.dmacnt = {}
        self.last = {}

    def _deps(self, eng, reads, writes):
        need = {}
        seen = self.seen[eng]

        def add(chan, gv):
            if chan == "pe" and eng == "pe":
                return
            s = seen.get(chan)
            if s is not None and s >= gv:
                return
            cur = need.get(chan)
            if cur is None or cur < gv:
                need[chan] = gv

        for b in reads:
            if b.w is not None:
                add(b.w[0], b.w[1])
        for b in writes:
            if b.w is not None:
                add(b.w[0], b.w[1])
            for chan, gv in b.r.items():
                add(chan, gv)
        waits = []
        for chan, gv in need.items():
            seen[chan] = gv
            key = (chan, gv[0])
            self.semkeys[key] = True
            waits.append((key, gv[1]))
        return waits

    def _mark(self, chan, gv, reads, writes):
        for b in reads:
            cur = b.r.get(chan)
            if cur is None or cur < gv:
                b.r[chan] = gv
        for b in writes:
            b.w = (chan, gv)
            b.r = {}
        self.last[chan] = gv

    def op(self, eng, fn, reads=(), writes=()):
        waits = self._deps(eng, reads, writes)
        if self.cnt[eng] >= self.ROT:
            self.gen[eng] += 1
            self.cnt[eng] = 0
        self.cnt[eng] += 1
        gv = (self.gen[eng], self.cnt[eng])
        key = (eng, gv[0])
        self.semkeys[key] = True
        self.ops[eng].append((waits, fn, (key, 1)))
        self._mark(eng, gv, reads, writes)

    def dma(self, queue, chan, fn, reads=(), writes=()):
        waits = self._deps(queue, reads, writes)
        n = self.dmacnt.get(chan, 0) + 1
        self.dmacnt[chan] = n
        gv = (0, 16 * n)
        key = (chan, 0)
        self.semkeys[key] = True
        self.ops[queue].append((waits, fn, (key, 16)))
        self._mark(chan, gv, reads, writes)

    def barrier(self, final=False):
        for eng in ENGS:
            waits = []
            seen = self.seen[eng]
            for chan, gv in self.last.items():
                if chan == "pe" and eng == "pe":
                    continue
                if chan.startswith("cv_") and not final:
                    continue
                s = seen.get(chan)
                if s is not None and s >= gv:
                    continue
                seen[chan] = gv
                waits.append(((chan, gv[0]), gv[1]))
            if waits:
                self.ops[eng].append((waits, None, None))

    def emit(self, nc, es):
        sems = {}
        for i, key in enumerate(self.semkeys):
            sems[key] = es.enter_context(nc.semaphore("s%d" % i))
        block = es.enter_context(nc.Block())

        def run(e, ops):
            for waits, fn, inc in ops:
                for key, val in waits:
                    e.wait_ge(sems[key], val)
                if fn is not None:
                    ins = fn(e)
                    ins.then_inc(sems[inc[0]], inc[1])

        @block.tensor
        def _(e):
            run(e, self.ops["pe"])

        @block.scalar
        def _(e):
            run(e, self.ops["act"])

        @block.vector
        def _(e):
            run(e, self.ops["dve"])

        @block.gpsimd
        def _(e):
            run(e, self.ops["pool"])

        @block.sync
        def _(e):
            run(e, self.ops["sp"])


def build_program(nseq=2, stop=4, sub=3, nheads=4, ngroups=4):
    nc = bass.Bass("TRN2", target_bir_lowering=False)
    P = Prog()
    es = ExitStack()

    def din(name, shape, dt=F32):
        return nc.dram_tensor(name, list(shape), dt, kind="ExternalInput").ap()

    def dscr(name, shape):
        return nc.dram_tensor(name, list(shape), BF16, kind="Internal").ap()

    x_d = din("x", [nseq, S, D])
    out_d = nc.dram_tensor("out", [nseq, S, D], F32, kind="ExternalOutput").ap()
    w0h_d = din("w0h", [4, D, 1536])
    wo0_d = din("wo0", [2048, D])
    wu_d = [din("wu0", [8, D, 512]), din("wu1", [8, D, 512])]
    wd_d = [din("wd0", [8, 512, D]), din("wd1", [8, 512, D])]
    w1h_d = din("w1h", [8, D, 384])
    wo1_d = din("wo1", [D, D])
    g8_d = din("g8", [8, 128, D])
    cs0_d = din("cs0", [2, 128, S])
    cs1_d = din("cs1", [2, 128, S])
    cmask_d = din("cmask", [128, 4 * 128 + 4 * 128 + 4 + 2 * 256 + 64 + 8 + 8])
    cbf_d = din("cbf", [128, 256])
    w0h_s = dscr("w0h_s", [4, D, 1536])
    wo0_s = dscr("wo0_s", [2048, D])
    wu_s = [dscr("wu0_s", [8, D, 512]), dscr("wu1_s", [8, D, 512])]
    wd_s = [dscr("wd0_s", [8, 512, D]), dscr("wd1_s", [8, 512, D])]
    w1h_s = dscr("w1h_s", [8, D, 384])
    wo1_s = dscr("wo1_s", [D, D])

    def sb(name, shape, dt):
        return es.enter_context(nc.sbuf_tensor(name, list(shape), dt))

    RH = sb("RH", [128, 32768], BF16)
    RX = sb("RX", [128, 16384], BF16)
    RO = sb("RO", [128, 32768], BF16)
    RS = sb("RS", [128, 12288], BF16)
    gbuf = sb("gbuf", [128, 2, D], F32)
    stats = sb("stats", [128, 256], F32)
    junk = sb("junk", [128, D], BF16)
    cmask = sb("cmask_sb", [128, 4 * 128 + 4 * 128 + 4 + 2 * 256 + 64 + 8 + 8], F32)
    cbf = sb("cbf_sb", [128, 256], BF16)
    PS = es.enter_context(nc.psum_tensor("PS", [128, 3072], F32))
    PSB = es.enter_context(nc.psum_tensor("PSB", [128, 2048], BF16))

    def view(reg, off_bytes, shape, dt):
        n = int(np.prod(shape))
        esz = 4 if dt == F32 else 2
        a = reg[:, off_bytes // 2: off_bytes // 2 + n * esz // 2]
        if dt == F32:
            a = a.bitcast(F32)
        if len(shape) == 2:
            a = a.rearrange("p (a b) -> p a b", a=shape[0])
        return a

    K = 1024
    maskT = cmask[:, 0:512].rearrange("p (h j) -> p h j", h=4)
    qdec = cmask[:, 512:1024].rearrange("p (h j) -> p h j", h=4)
    kdec = cmask[:, 1024:1028]
    causal = cmask[:, 1028:1540].rearrange("p (a j) -> p a j", a=2)
    pastm = cmask[:, 1540:1604].rearrange("p (b n) -> p b n", b=8)
    neghalf = cmask[:, 1604:1612]
    zeros8 = cmask[:, 1612:1620]
    ident = cbf[:, 0:128]
    Rmat = cbf[:, 128:256]

    PF = [PS[:, i * 512:(i + 1) * 512] for i in range(6)]
    PB = [PSB[:, i * 1024:(i + 1) * 1024] for i in range(2)]
    pfb = [Buf() for _ in range(6)]
    pbb = [Buf() for _ in range(2)]
    constb = Buf()

    hview = RH[:, :].bitcast(F32).rearrange("p (t d) -> p t d", t=NT)
    hb = [Buf() for _ in range(NT)]
    xnT = RX[:, :].rearrange("p (c t) -> p c t", c=8)
    xnb = [Buf() for _ in range(NT)]

    st_state = {"i": 0}
    stb = [Buf() for _ in range(32)]

    def stat():
        i = st_state["i"] % 32
        st_state["i"] += 1
        return stats[:, i * 8:(i + 1) * 8], stb[i]

    gb = [Buf(), Buf()]
    g_state = {"i": 0}

    def load_gain(idx):
        s = g_state["i"] % 2
        g_state["i"] += 1
        P.dma("sp", "gain%d" % s, lambda e, s=s, idx=idx: e.dma_start(out=gbuf[:, s, :], in_=g8_d[idx]),
              writes=[gb[s]])
        return gbuf[:, s, :], gb[s]

    cvb = {}

    def convert(name, src, dst, nchunk):
        b = Buf()
        cvb[name] = b
        tot = int(np.prod(src.shape))
        per = tot // nchunk
        sflat = src.rearrange(" ".join("abcd"[:src.ndim]) + " -> (" + " ".join("abcd"[:src.ndim]) + ")")
        dflat = dst.rearrange(" ".join("abcd"[:dst.ndim]) + " -> (" + " ".join("abcd"[:dst.ndim]) + ")")
        for c in range(nchunk):
            si = sflat[c * per:(c + 1) * per].rearrange("(p n) -> p n", p=128)
            di = dflat[c * per:(c + 1) * per].rearrange("(p n) -> p n", p=128)
            P.dma("pool", "cv_" + name, lambda e, si=si, di=di: e.dma_start(out=di, in_=si), writes=[b])

    def rstd_from_ss(ss_ap, ssb, n, out_ap, outb, tmp_ap):
        P.op("pool", lambda e: e.tensor_scalar(out=tmp_ap, in0=ss_ap, scalar1=1.0 / n, scalar2=EPS,
                                               op0=ALU.mult, op1=ALU.add), reads=[ssb], writes=[outb])
        P.op("pool", lambda e: e.tensor_tensor(out=out_ap, in0=tmp_ap, in1=neghalf[:, 0:1], op=ALU.pow),
             reads=[outb, constb], writes=[outb])

    def prenorm(tiles, src_fn, gain_ap, gain_b, dstT, dst_bufs, xs_view, xs_bufs):
        for n, i in enumerate(tiles):
            src, srcb = src_fn(i)
            st, stbuf = stat()
            P.op("act", lambda e, src=src, st=st: e.activation(out=junk[:, :], in_=src, func=AF.Square,
                                                                accum_out=st[:, 0:1]),
                 reads=[srcb], writes=[stbuf])
            rstd_from_ss(st[:, 0:1], stbuf, D, st[:, 2:3], stbuf, st[:, 1:2])
            xs = xs_view[:, n % 2, :]
            xsb = xs_bufs[n % 2]
            P.op("dve", lambda e, src=src, st=st, xs=xs: e.scalar_tensor_tensor(
                out=xs, in0=src, scalar=st[:, 2:3], in1=gain_ap, op0=ALU.mult, op1=ALU.mult),
                reads=[srcb, stbuf, gain_b], writes=[xsb])
            pb = n % 2

            def tr(e, xs=xs, pb=pb):
                ins = None
                for c in range(8):
                    ins = e.transpose(out=PB[pb][:, c * 128:(c + 1) * 128], in_=xs[:, c * 128:(c + 1) * 128],
                                      identity=ident)
                return ins
            P.op("pe", tr, reads=[xsb, constb], writes=[pbb[pb]])
            dst = dstT[:, :, n * 128:(n + 1) * 128]
            P.op("act", lambda e, dst=dst, pb=pb: e.activation(
                out=dst, in_=PB[pb].rearrange("p (c t) -> p c t", c=8), func=AF.Copy),
                reads=[pbb[pb]], writes=[dst_bufs[n]])

    def postnorm_tile(y_ap, y_bufs, gain_ap, gain_b, t, tmp_ap, tmp_b):
        st, stbuf = stat()
        for hf in range(2):
            hs = slice(hf * 512, (hf + 1) * 512)
            P.op("act", lambda e, hf=hf, hs=hs: e.activation(out=junk[:, hs], in_=y_ap[:, hs], func=AF.Square,
                                                              accum_out=st[:, 3 + hf:4 + hf]),
                 reads=[y_bufs[hf % len(y_bufs)]], writes=[stbuf])
        P.op("pool", lambda e: e.tensor_tensor(out=st[:, 0:1], in0=st[:, 3:4], in1=st[:, 4:5], op=ALU.add),
             reads=[stbuf], writes=[stbuf])
        rstd_from_ss(st[:, 0:1], stbuf, D, st[:, 2:3], stbuf, st[:, 1:2])
        for hf in range(2):
            hs = slice(hf * 512, (hf + 1) * 512)
            P.op("dve", lambda e, hs=hs: e.scalar_tensor_tensor(out=tmp_ap[:, hs], in0=y_ap[:, hs], scalar=st[:, 2:3],
                                                                in1=gain_ap[:, hs], op0=ALU.mult, op1=ALU.mult),
                 reads=[y_bufs[hf % len(y_bufs)], stbuf, gain_b], writes=[tmp_b])
        P.op("pool", lambda e: e.tensor_tensor(out=hview[:, t, :], in0=hview[:, t, :], in1=tmp_ap, op=ALU.add),
             reads=[tmp_b, hb[t]], writes=[hb[t]])

    def out_proj(srcT, src_bufs_fn, nk, w_ap, wbuf, gain_ap, gain_b, tmp_view, tmp_bufs):
        for t in range(NT):
            yb = t % 2
            ybufs = [pfb[2 * yb], pfb[2 * yb + 1]]
            y_ap = PS[:, yb * 1024:(yb + 1) * 1024]

            def mm(e, t=t, yb=yb):
                ins = None
                for half in range(2):
                    for k in range(nk):
                        ins = e.matmul(PF[2 * yb + half], lhsT=srcT[:, k, t * 128:(t + 1) * 128],
                                       rhs=w_ap[:, k, half * 512:(half + 1) * 512],
                                       start=(k == 0), stop=(k == nk - 1))
                return ins
            P.op("pe", mm, reads=src_bufs_fn(t) + [wbuf], writes=ybufs)
            postnorm_tile(y_ap, ybufs, gain_ap, gain_b, t, tmp_view[:, t % 2, :], tmp_bufs[t % 2])

    def mlp(layer, final, s):
        wu_sb = [view(RO, 32768 + j * 8192, [8, 512], BF16) for j in range(2)]
        wd_sb = [view(RO, 49152 + j * 8192, [4, 1024], BF16) for j in range(2)]
        wub = [Buf(), Buf()]
        wdb = [Buf(), Buf()]
        ysb = view(RO, 0, [8, 1024], F32)
        yb_ = [Buf() for _ in range(8)]
        xn2T = RX[:, 0:8192].rearrange("p (c t) -> p c t", c=8)
        xn2b = [Buf() for _ in range(8)]
        xs_view = view(RS, 0, [2, 1024], BF16)
        xs_bufs = [Buf(), Buf()]
        r_v = view(RS, 4096, [2, 512], F32)
        r_b = [Buf(), Buf()]
        uT = [view(RS, 16384 + g * 4096, [4, 512], BF16) for g in range(2)]
        uTb = [[Buf() for _ in range(4)] for _ in range(2)]
        tmp_view = view(RS, 8192, [2, 1024], F32)
        tmp_bufs = [Buf(), Buf()]
        yb_ = [[Buf(), Buf()] for _ in range(8)]
        for half in range(2):
            gpre, gpreb = load_gain(layer * 4 + 2)
            prenorm([half * 8 + i for i in range(8)], lambda t: (hview[:, t, :], hb[t]), gpre, gpreb,
                    xn2T, xn2b, xs_view, xs_bufs)
            uc = 0
            yc = 0
            ugc = 0
            rc = 0
            for j in range(8):
                sl = j % 2
                P.dma("sp", "wu%d" % sl, lambda e, sl=sl, j=j: e.dma_start(
                    out=wu_sb[sl], in_=wu_s[layer][j].rearrange("(k p) c -> p k c", p=128)),
                    reads=[cvb["wu%d_%d" % (layer, j)]], writes=[wub[sl]])
                P.dma("sp", "wd%d" % sl, lambda e, sl=sl, j=j: e.dma_start(
                    out=wd_sb[sl], in_=wd_s[layer][j].rearrange("(c p) n -> p c n", p=128)),
                    reads=[cvb["wd%d_%d" % (layer, j)]], writes=[wdb[sl]])
                for tg in range(2):
                    ug = ugc % 2
                    ugc += 1
                    for ffc in range(4):
                        ub = uc % 3
                        uc += 1
                        rb = rc % 2
                        rc += 1

                        def mmu(e, sl=sl, ffc=ffc, tg=tg, ub=ub):
                            ins = None
                            for k in range(8):
                                ins = e.matmul(PF[ub], lhsT=wu_sb[sl][:, k, ffc * 128:(ffc + 1) * 128],
                                               rhs=xn2T[:, k, tg * 512:(tg + 1) * 512],
                                               start=(k == 0), stop=(k == 7))
                            return ins
                        P.op("pe", mmu, reads=[wub[sl]] + xn2b[4 * tg:4 * tg + 4], writes=[pfb[ub]])
                        P.op("act", lambda e, ub=ub, rb=rb: e.activation(out=r_v[:, rb, :], in_=PF[ub], func=AF.Relu),
                             reads=[pfb[ub]], writes=[r_b[rb]])
                        P.op("dve", lambda e, rb=rb, ug=ug, ffc=ffc: e.tensor_tensor(
                            out=uT[ug][:, ffc, :], in0=r_v[:, rb, :], in1=r_v[:, rb, :], op=ALU.mult),
                            reads=[r_b[rb]], writes=[uTb[ug][ffc]])
                    for tt in range(4):
                        ti = 4 * tg + tt
                        for h2 in range(2):
                            yb = 3 + yc % 3
                            yc += 1
                            hs = slice(h2 * 512, (h2 + 1) * 512)

                            def mmd(e, sl=sl, ug=ug, tt=tt, hs=hs, yb=yb):
                                ins = None
                                for ffc in range(4):
                                    ins = e.matmul(PF[yb], lhsT=uT[ug][:, ffc, tt * 128:(tt + 1) * 128],
                                                   rhs=wd_sb[sl][:, ffc, hs], start=(ffc == 0), stop=(ffc == 3))
                                return ins
                            P.op("pe", mmd, reads=uTb[ug] + [wdb[sl]], writes=[pfb[yb]])
                            if j == 0:
                                P.op("dve", lambda e, ti=ti, yb=yb, hs=hs: e.tensor_copy(out=ysb[:, ti, hs], in_=PF[yb]),
                                     reads=[pfb[yb]], writes=[yb_[ti][h2]])
                            else:
                                P.op("dve", lambda e, ti=ti, yb=yb, hs=hs: e.tensor_tensor(
                                    out=ysb[:, ti, hs], in0=PF[yb], in1=ysb[:, ti, hs], op=ALU.add),
                                    reads=[pfb[yb], yb_[ti][h2]], writes=[yb_[ti][h2]])
            gpost, gpostb = load_gain(layer * 4 + 3)
            for i in range(8):
                t = half * 8 + i
                postnorm_tile(ysb[:, i, :], yb_[i], gpost, gpostb, t, tmp_view[:, i % 2, :], tmp_bufs[i % 2])
                if final:
                    P.dma("sp", "outd%d" % t, lambda e, t=t: e.dma_start(out=out_d[s, t * 128:(t + 1) * 128, :],
                                                                  in_=hview[:, t, :]), reads=[hb[t]])

    CD = [float((1.0 - 2.0 ** (-5.0 - h)) ** 128) for h in range(4)]

    def layer0(s, sub=3, nheads=4, ngroups=4):
        xin = view(RS, 0, [2, 1024], F32)
        xinb = [Buf(), Buf()]
        xs_view = view(RS, 8192, [2, 1024], BF16)
        xs_bufs = [Buf(), Buf()]
        gpre, gpreb = load_gain(0)

        def src_fn(t):
            sl = t % 2
            P.dma("sp", "xin%d" % sl, lambda e, sl=sl, t=t: e.dma_start(out=xin[:, sl, :],
                                                                      in_=x_d[s, t * 128:(t + 1) * 128, :]),
                  writes=[xinb[sl]])
            return xin[:, sl, :], xinb[sl]
        prenorm(list(range(NT)), src_fn, gpre, gpreb, xnT, xnb, xs_view, xs_bufs)
        P.barrier()
        if sub == 1:
            for t in range(NT):
                P.dma("sp", "xh%d" % t, lambda e, t=t: e.dma_start(out=hview[:, t, :], in_=x_d[s, t * 128:(t + 1) * 128, :]),
                      writes=[hb[t]])
            return
        wh = [view(RH, j * 24576, [8, 1536], BF16) for j in range(2)]
        whb = [Buf(), Buf()]
        cs = view(RH, 49152, [2, S], F32)
        csb = Buf()
        P.dma("sp", "cs", lambda e: e.dma_start(out=cs[:, 0, :], in_=cs0_d[0]), writes=[csb])
        P.dma("sp", "cs", lambda e: e.dma_start(out=cs[:, 1, :], in_=cs0_d[1]), writes=[csb])
        ogT = RO[:, :].rearrange("p (c t) -> p c t", c=16)
        ogb = [Buf() for _ in range(NT)]
        S32 = view(RS, 0, [2, 512], F32)
        Sbf = view(RS, 4096, [2, 512], BF16)
        s32b, sbfb = Buf(), Buf()
        qT = view(RS, 6144, [2, 512], BF16)
        kT = view(RS, 8192, [2, 512], BF16)
        qsT = view(RS, 10240, [2, 512], BF16)
        qTb, kTb, qsTb = Buf(), Buf(), Buf()
        tt_ = [view(RS, 12288 + i * 2048, [512], F32) for i in range(4)]
        ttb = [Buf() for _ in range(4)]
        gb16 = gbuf[:, 0, :].bitcast(BF16)
        v_bfs = [view(RS, 20480, [512], BF16), gb16[:, 0:512]]
        sgs = [view(RS, 21504, [512], BF16), gb16[:, 512:1024]]
        vbs, sgbs = [Buf(), Buf()], [Buf(), Buf()]
        sT_bf = view(RS, 22528, [128], BF16)
        kd_bf = view(RS, 22784, [256], BF16)
        og_bf = view(RS, 23296, [512], BF16)
        sTb, kdb, ogbf = Buf(), Buf(), Buf()
        def proj_vg(ci, w, sl):
            tk = slice(ci * 128, (ci + 1) * 128)
            v_bf, sg, vb, sgb = v_bfs[ci % 2], sgs[ci % 2], vbs[ci % 2], sgbs[ci % 2]

            def mmv(e):
                ins = None
                for k in range(8):
                    ins = e.matmul(PF[0], lhsT=xnT[:, k, tk], rhs=w[:, k, 512:1024], start=(k == 0), stop=(k == 7))
                return ins
            P.op("pe", mmv, reads=[whb[sl], xnb[ci]], writes=[pfb[0]])
            P.op("act", lambda e: e.activation(out=v_bf, in_=PF[0], func=AF.Copy), reads=[pfb[0]], writes=[vb])

            def mmg(e):
                ins = None
                for k in range(8):
                    ins = e.matmul(PF[1], lhsT=xnT[:, k, tk], rhs=w[:, k, 1024:1536], start=(k == 0), stop=(k == 7))
                return ins
            P.op("pe", mmg, reads=[whb[sl], xnb[ci]], writes=[pfb[1]])
            P.op("act", lambda e: e.activation(out=sg, in_=PF[1], func=AF.Silu), reads=[pfb[1]], writes=[sgb])

        for h in range(nheads):
            sl = h % 2
            if s == 0 and h == min(2, nheads - 1):
                P.barrier(final=True)
            P.dma("sp", "wh%d" % sl, lambda e, sl=sl, h=h: e.dma_start(
                out=wh[sl], in_=w0h_s[h].rearrange("(k p) c -> p k c", p=128)),
                reads=[cvb["w0h_%d" % h]], writes=[whb[sl]])
            w = wh[sl]
            for G in range(ngroups):
                gs = slice(G * 512, (G + 1) * 512)
                banks = [0, 1, 4, 5]
                for fc in range(4):
                    def mmqk(e, fc=fc, w=w, gs=gs):
                        ins = None
                        for k in range(8):
                            ins = e.matmul(PF[banks[fc]], lhsT=w[:, k, fc * 128:(fc + 1) * 128],
                                           rhs=xnT[:, k, gs], start=(k == 0), stop=(k == 7))
                        return ins
                    P.op("pe", mmqk, reads=[whb[sl]] + xnb[G * 4:(G + 1) * 4], writes=[pfb[banks[fc]]])
                cosg = cs[:, 0, gs]
                sing = cs[:, 1, gs]
                for which, (b1, b2, dstT, dstb) in enumerate(((0, 1, qT, qTb), (4, 5, kT, kTb))):
                    x1, x2 = PF[b1], PF[b2]
                    P.op("dve", lambda e, x1=x1, cosg=cosg: e.tensor_tensor(out=tt_[0], in0=x1, in1=cosg, op=ALU.mult),
                         reads=[pfb[b1], csb], writes=[ttb[0]])
                    P.op("dve", lambda e, x2=x2, sing=sing: e.tensor_tensor(out=tt_[1], in0=x2, in1=sing, op=ALU.mult),
                         reads=[pfb[b2], csb], writes=[ttb[1]])
                    P.op("pool", lambda e, dstT=dstT: e.tensor_tensor(out=dstT[:, 0, :], in0=tt_[0], in1=tt_[1],
                                                                      op=ALU.subtract),
                         reads=[ttb[0], ttb[1]], writes=[dstb])
                    P.op("dve", lambda e, x1=x1, sing=sing: e.tensor_tensor(out=tt_[2], in0=x1, in1=sing, op=ALU.mult),
                         reads=[pfb[b1], csb], writes=[ttb[2]])
                    P.op("dve", lambda e, x2=x2, cosg=cosg: e.tensor_tensor(out=tt_[3], in0=x2, in1=cosg, op=ALU.mult),
                         reads=[pfb[b2], csb], writes=[ttb[3]])
                    P.op("pool", lambda e, dstT=dstT: e.tensor_tensor(out=dstT[:, 1, :], in0=tt_[2], in1=tt_[3],
                                                                      op=ALU.add),
                         reads=[ttb[2], ttb[3]], writes=[dstb])
                P.op("pool", lambda e, h=h: e.tensor_tensor(
                    out=qsT.rearrange("p a (c i) -> p (a c) i", i=128),
                    in0=qT.rearrange("p a (c i) -> p (a c) i", i=128),
                    in1=qdec[:, h:h + 1, :].to_broadcast([128, 8, 128]), op=ALU.mult),
                    reads=[qTb, constb], writes=[qsTb])
                for c in range(4):
                    ci = G * 4 + c
                    tk = slice(ci * 128, (ci + 1) * 128)
                    tg = slice(c * 128, (c + 1) * 128)

                    v_bf, sg, vb, sgb = v_bfs[ci % 2], sgs[ci % 2], vbs[ci % 2], sgbs[ci % 2]
                    if ci == 0:
                        proj_vg(0, w, sl)

                    def mms(e, tg=tg):
                        ins = None
                        for dc in range(2):
                            ins = e.matmul(PF[2][:, 0:128], lhsT=kT[:, dc, tg], rhs=qT[:, dc, tg],
                                           start=(dc == 0), stop=(dc == 1))
                        return ins
                    P.op("pe", mms, reads=[qTb, kTb], writes=[pfb[2]])
                    P.op("dve", lambda e, h=h: e.tensor_tensor(out=sT_bf, in0=PF[2][:, 0:128], in1=maskT[:, h, :],
                                                               op=ALU.mult),
                         reads=[pfb[2], constb], writes=[sTb])

                    def trk(e, tg=tg):
                        ins = None
                        for dc in range(2):
                            ins = e.transpose(out=PB[0][:, dc * 128:(dc + 1) * 128], in_=kT[:, dc, tg], identity=ident)
                        return ins
                    P.op("pe", trk, reads=[kTb, constb], writes=[pbb[0]])
                    P.op("dve", lambda e, h=h: e.tensor_scalar(out=kd_bf, in0=PB[0][:, 0:256], scalar1=kdec[:, h:h + 1],
                                                               scalar2=None, op0=ALU.mult),
                         reads=[pbb[0], constb], writes=[kdb])

                    def mmo(e, ci=ci, tg=tg, v_bf=v_bf):
                        ins = e.matmul(PF[3], lhsT=sT_bf, rhs=v_bf, start=True, stop=(ci == 0))
                        if ci > 0:
                            for dc in range(2):
                                ins = e.matmul(PF[3], lhsT=qsT[:, dc, tg], rhs=Sbf[:, dc, :], start=False, stop=(dc == 1))
                        return ins
                    P.op("pe", mmo, reads=[sTb, vb, qsTb, sbfb], writes=[pfb[3]])
                    if ci < 15:
                        for dc in range(2):
                            P.op("pe", lambda e, dc=dc, v_bf=v_bf: e.matmul(PF[4 + dc], lhsT=kd_bf[:, dc * 128:(dc + 1) * 128],
                                                                             rhs=v_bf, start=True, stop=True),
                                 reads=[kdb, vb], writes=[pfb[4 + dc]])
                    st, stbuf = stat()
                    P.op("act", lambda e, st=st: e.activation(out=junk[:, 0:512], in_=PF[3], func=AF.Square,
                                                               accum_out=st[:, 0:1]),
                         reads=[pfb[3]], writes=[stbuf])
                    rstd_from_ss(st[:, 0:1], stbuf, 512, st[:, 2:3], stbuf, st[:, 1:2])
                    if ci < 15:
                        proj_vg(ci + 1, w, sl)
                    P.op("dve", lambda e, st=st, sg=sg: e.scalar_tensor_tensor(out=og_bf, in0=PF[3], scalar=st[:, 2:3], in1=sg,
                                                                               op0=ALU.mult, op1=ALU.mult),
                         reads=[pfb[3], stbuf, sgb], writes=[ogbf])

                    def tro(e):
                        ins = None
                        for ec in range(4):
                            ins = e.transpose(out=PB[1][:, ec * 128:(ec + 1) * 128], in_=og_bf[:, ec * 128:(ec + 1) * 128],
                                              identity=ident)
                        return ins
                    P.op("pe", tro, reads=[ogbf, constb], writes=[pbb[1]])
                    P.op("act", lambda e, h=h, tk=tk: e.activation(
                        out=ogT[:, h * 4:(h + 1) * 4, tk], in_=PB[1][:, 0:512].rearrange("p (c t) -> p c t", c=4),
                        func=AF.Copy), reads=[pbb[1]], writes=[ogb[ci]])
                    if ci < 15:
                        for dc in range(2):
                            if ci == 0:
                                P.op("dve", lambda e, dc=dc: e.tensor_copy(out=S32[:, dc, :], in_=PF[4 + dc]),
                                     reads=[pfb[4 + dc]], writes=[s32b])
                            else:
                                P.op("dve", lambda e, dc=dc, h=h: e.scalar_tensor_tensor(
                                    out=S32[:, dc, :], in0=S32[:, dc, :], scalar=CD[h], in1=PF[4 + dc],
                                    op0=ALU.mult, op1=ALU.add), reads=[pfb[4 + dc], s32b], writes=[s32b])
                        P.op("pool", lambda e: e.tensor_copy(out=Sbf, in_=S32), reads=[s32b], writes=[sbfb])
        P.barrier()
        if sub == 2:
            for t in range(NT):
                P.dma("sp", "xh%d" % t, lambda e, t=t: e.dma_start(out=hview[:, t, :], in_=x_d[s, t * 128:(t + 1) * 128, :]),
                      writes=[hb[t]])
            return
        wo = RX[:, :].rearrange("p (k c) -> p k c", k=16)
        wob = Buf()
        P.dma("sp", "wo", lambda e: e.dma_start(out=wo, in_=wo0_s.rearrange("(k p) c -> p k c", p=128)),
              reads=[cvb["wo0"]], writes=[wob])
        for t in range(NT):
            P.dma("sp", "xh%d" % t, lambda e, t=t: e.dma_start(out=hview[:, t, :], in_=x_d[s, t * 128:(t + 1) * 128, :]),
                  writes=[hb[t]])
        gpost, gpostb = load_gain(1)
        tmp_view = view(RS, 0, [2, 1024], F32)
        tmp_bufs = [Buf(), Buf()]
        out_proj(ogT, lambda t: [ogb[t]], 16, wo, wob, gpost, gpostb, tmp_view, tmp_bufs)
        P.barrier()

    SC = float(128 ** -0.5)

    def layer1(s):
        import os
        sub1 = int(os.environ.get('L1SUB', 9))
        nh1 = int(os.environ.get('L1NH', 8))
        nt1 = int(os.environ.get('L1NT', 16))
        l1p = int(os.environ.get('L1P', 63))
        xs_view = view(RS, 0, [2, 1024], BF16)
        xs_bufs = [Buf(), Buf()]
        gpre, gpreb = load_gain(4)
        prenorm(list(range(NT)), lambda t: (hview[:, t, :], hb[t]), gpre, gpreb, xnT, xnb, xs_view, xs_bufs)
        P.barrier()
        if sub1 == 1:
            return
        oT = RO[:, 0:16384].rearrange("p (c t) -> p c t", c=8)
        oTb = [Buf() for _ in range(NT)]
        cs = view(RO, 32768, [2, S], F32)
        csb = Buf()
        P.dma("sp", "cs", lambda e: e.dma_start(out=cs[:, 0, :], in_=cs1_d[0]), writes=[csb])
        P.dma("sp", "cs", lambda e: e.dma_start(out=cs[:, 1, :], in_=cs1_d[1]), writes=[csb])
        wh = [view(RO, 49152 + j * 6144, [8, 384], BF16) for j in range(2)]
        whb = [Buf(), Buf()]
        t1 = view(RO, 61440, [512], F32)
        t2 = view(RO, 63488, [512], F32)
        if int(os.environ.get('E2', 0)) == 1:
            t1 = gbuf[:, 0, 0:512]
            t2 = gbuf[:, 0, 512:1024]
        if int(os.environ.get('E2', 0)) == 2:
            t1 = view(RO, 55296, [512], F32)
            t2 = view(RO, 57344, [512], F32)
        if int(os.environ.get('E2', 0)) == 3:
            t1 = view(RO, 59392, [512], F32)
            t2 = view(RO, 61440, [512], F32)
        t1b, t2b = Buf(), Buf()
        qT = view(RS, 0, [S], BF16)
        kT = view(RS, 4096, [S], BF16)
        vh = view(RS, 8192, [16, 128], BF16)
        p_bf = view(RS, 12288, [S], BF16)
        pT = view(RS, 16384, [16, 128], BF16)
        sown = view(RS, 20480, [256], F32)
        o_bf = view(RS, 21504, [128], BF16)
        kmT = view(RS, 21760, [8], BF16)
        ksum = view(RS, 21776, [8], F32)
        qTb = [Buf() for _ in range(4)]
        kTb = [Buf() for _ in range(4)]
        vhb, pTb, obfb, kmb = Buf(), Buf(), Buf(), Buf()
        pbufs = [p_bf, gbuf[:, 1, :].bitcast(BF16)]
        pbs = [Buf(), Buf()]
        sowns = [sown, gbuf[:, 0, 0:256]]
        sownbs = [Buf(), Buf()]
        unit_ctr = [0]
        for h in range(nh1):
            sl = h % 2
            P.dma("sp", "wh%d" % sl, lambda e, sl=sl, h=h: e.dma_start(
                out=wh[sl], in_=w1h_s[h].rearrange("(k p) c -> p k c", p=128)),
                reads=[cvb["w1h"]], writes=[whb[sl]])
            w = wh[sl]
            for which, (dst, dstb) in enumerate(((qT, qTb), (kT, kTb))):
                for G in range(4):
                    gs = slice(G * 512, (G + 1) * 512)

                    def mmp(e, w=w, gs=gs, which=which, G=G):
                        ins = None
                        for k in range(8):
                            ins = e.matmul(PF[G], lhsT=w[:, k, which * 128:(which + 1) * 128], rhs=xnT[:, k, gs],
                                           start=(k == 0), stop=(k == 7))
                        return ins
                    P.op("pe", mmp, reads=[whb[sl]] + xnb[G * 4:(G + 1) * 4], writes=[pfb[G]])
                    P.op("act", lambda e, dst=dst, gs=gs, G=G: e.activation(out=dst[:, gs], in_=PF[G], func=AF.Copy),
                         reads=[pfb[G]], writes=[dstb[G]])
                    if not (l1p & 2):
                        continue
                    P.op("pe", lambda e, dst=dst, gs=gs: e.matmul(PF[4], lhsT=Rmat, rhs=dst[:, gs],
                                                                   start=True, stop=True),
                         reads=[dstb[G], constb], writes=[pfb[4]])
                    if not (l1p & 16):
                        continue
                    P.op("dve", lambda e, G=G, gs=gs: e.tensor_tensor(out=t1, in0=PF[G], in1=cs[:, 0, gs],
                                                                      op=ALU.mult),
                         reads=[pfb[G], csb, dstb[G]], writes=[t1b])
                    P.op("dve", lambda e, gs=gs: e.tensor_tensor(out=t2, in0=PF[4], in1=cs[:, 1, gs],
                                                                 op=ALU.mult),
                         reads=[pfb[4], csb], writes=[t2b])
                    if not (l1p & 32):
                        continue
                    P.op("pool", lambda e, dst=dst, gs=gs: e.tensor_tensor(out=dst[:, gs], in0=t1, in1=t2, op=ALU.add),
                         reads=[t1b, t2b], writes=[dstb[G]])
            for t4 in range(4 if (l1p & 4) else 0):
                def mmv(e, w=w, t4=t4):
                    ins = None
                    for tt in range(4):
                        t = t4 * 4 + tt
                        for k in range(8):
                            ins = e.matmul(PF[5][:, tt * 128:(tt + 1) * 128], lhsT=xnT[:, k, t * 128:(t + 1) * 128],
                                           rhs=w[:, k, 256:384], start=(k == 0), stop=(k == 7))
                    return ins
                P.op("pe", mmv, reads=[whb[sl]] + xnb[t4 * 4:(t4 + 1) * 4], writes=[pfb[5]])
                P.op("act", lambda e, t4=t4: e.activation(out=vh[:, t4 * 4:(t4 + 1) * 4, :],
                                                          in_=PF[5].rearrange("p (a b) -> p a b", a=4), func=AF.Copy),
                     reads=[pfb[5]], writes=[vhb])
            if l1p & 8:
                P.op("dve", lambda e: e.tensor_reduce(out=ksum, in_=kT.rearrange("p (n j) -> p n j", n=8), axis=AX.X,
                                                      op=ALU.add), reads=kTb, writes=[kmb])
                P.op("dve", lambda e: e.tensor_copy(out=kmT, in_=ksum), reads=[kmb], writes=[kmb])
            def unitA(t, par, ctx):
                b = t // 2
                odd = t % 2
                tq = slice(t * 128, (t + 1) * 128)
                nown = 128 * (1 + odd)
                nkeys = b * 256 + nown
                nkt = nkeys // 128
                nbank = (nkeys + 511) // 512
                base = 2 * par if nbank <= 2 else 0
                bk = lambda j: base + j
                p_cur, pcb = pbufs[par], pbs[par]
                so, sob = sowns[par], sownbs[par]
                qbuf = [qTb[t // 4]]
                for j in range(nbank):
                    wdt = min(512, nkeys - j * 512)
                    P.op("pe", lambda e, j=j, wdt=wdt, bj=bk(j): e.matmul(PF[bj][:, 0:wdt], lhsT=qT[:, tq],
                                                                          rhs=kT[:, j * 512:j * 512 + wdt],
                                                                          start=True, stop=True),
                         reads=qbuf + kTb, writes=[pfb[bk(j)]])
                st, stbuf = stat()
                st2, stbuf2 = stat()
                st3, stbuf3 = stat()
                if b >= 4:
                    P.op("pe", lambda e: e.matmul(PF[4][:, 0:8], lhsT=qT[:, tq], rhs=kmT, start=True, stop=True),
                         reads=qbuf + [kmb], writes=[pfb[4]])
                    P.op("dve", lambda e: e.tensor_tensor(out=st, in0=PF[4][:, 0:8], in1=pastm[:, b, :], op=ALU.add),
                         reads=[pfb[4], constb], writes=[stbuf])
                    stm, stmb = stat()
                    P.op("dve", lambda e: e.max(out=stm, in_=st), reads=[stbuf], writes=[stmb])
                    P.op("dve", lambda e: e.tensor_scalar(out=st, in0=st, scalar1=stm[:, 2:3], scalar2=None, op0=ALU.is_ge),
                         reads=[stbuf, stmb], writes=[stbuf])
                    P.op("dve", lambda e: e.tensor_scalar(out=st, in0=st, scalar1=-1.0, scalar2=BIG, op0=ALU.add,
                                                          op1=ALU.mult), reads=[stbuf], writes=[stbuf])
                    selb = st
                    selbuf = stbuf
                else:
                    selb = zeros8
                    selbuf = constb
                jo, oo = divmod(b * 256, 512)
                own_ps = PF[bk(jo)][:, oo:oo + nown]
                P.op("dve", lambda e: e.tensor_tensor(out=so[:, 0:nown], in0=own_ps, in1=causal[:, odd, 0:nown], op=ALU.add),
                     reads=[pfb[bk(jo)], constb], writes=[sob])
                P.op("dve", lambda e: e.tensor_reduce(out=st2[:, b:b + 1], in_=so[:, 0:nown], axis=AX.X, op=ALU.max),
                     reads=[sob], writes=[stbuf2])
                for j in range((b * 256 + 511) // 512):
                    nb_here = min(2, b - 2 * j)
                    P.op("dve", lambda e, j=j, nb_here=nb_here, bj=bk(j): e.tensor_reduce(
                        out=st2[:, 2 * j:2 * j + nb_here],
                        in_=PF[bj][:, 0:nb_here * 256].rearrange("p (n k) -> p n k", n=nb_here), axis=AX.X, op=ALU.max),
                        reads=[pfb[bk(j)]], writes=[stbuf2])
                yield
                if b > 0:
                    P.op("dve", lambda e: e.tensor_tensor(out=st2[:, 0:b], in0=st2[:, 0:b], in1=selb[:, 0:b], op=ALU.add),
                         reads=[stbuf2, selbuf], writes=[stbuf2])
                stn, stnb = stat()
                P.op("dve", lambda e: e.tensor_reduce(out=stn[:, 0:1], in_=st2[:, 0:b + 1], axis=AX.X, op=ALU.max),
                     reads=[stbuf2], writes=[stnb])
                P.op("dve", lambda e: e.tensor_scalar(out=stn[:, 1:2], in0=stn[:, 0:1], scalar1=-SC, scalar2=None,
                                                      op0=ALU.mult), reads=[stnb], writes=[stnb])
                if b > 0:
                    P.op("dve", lambda e: e.tensor_scalar(out=st2[:, 0:b], in0=selb[:, 0:b], scalar1=stn[:, 1:2],
                                                          scalar2=None, op0=ALU.add),
                         reads=[selbuf, stnb, stbuf2], writes=[stbuf2])
                yield
                for n in range(b):
                    j, o = divmod(n * 256, 512)
                    P.op("act", lambda e, n=n, o=o, bj=bk(j): e.activation(
                        out=p_cur[:, n * 256:(n + 1) * 256], in_=PF[bj][:, o:o + 256], func=AF.Exp,
                        bias=st2[:, n:n + 1], scale=SC, accum_out=st3[:, n:n + 1]),
                        reads=[pfb[bk(j)], stbuf2], writes=[pcb, stbuf3])
                P.op("act", lambda e: e.activation(
                    out=p_cur[:, b * 256:b * 256 + nown], in_=so[:, 0:nown], func=AF.Exp,
                    bias=stn[:, 1:2], scale=SC, accum_out=st3[:, b:b + 1]),
                    reads=[sob, stnb], writes=[pcb, stbuf3])
                P.op("dve", lambda e: e.tensor_reduce(out=stn[:, 2:3], in_=st3[:, 0:b + 1], axis=AX.X, op=ALU.add),
                     reads=[stbuf3, stnb], writes=[stnb])
                P.op("dve", lambda e: e.reciprocal(out=stn[:, 3:4], in_=stn[:, 2:3]), reads=[stnb], writes=[stnb])
                ctx.update(t=t, tq=tq, nkt=nkt, p_cur=p_cur, pcb=pcb, stn=stn, stnb=stnb)

            def unitB(c):
                t, tq, nkt, p_cur, pcb, stn, stnb = c["t"], c["tq"], c["nkt"], c["p_cur"], c["pcb"], c["stn"], c["stnb"]
                for pbk in range((nkt + 7) // 8):
                    n_here = min(8, nkt - pbk * 8)

                    def trp(e, pbk=pbk, n_here=n_here):
                        ins = None
                        for i in range(n_here):
                            kt = pbk * 8 + i
                            ins = e.transpose(out=PB[pbk][:, i * 128:(i + 1) * 128], in_=p_cur[:, kt * 128:(kt + 1) * 128],
                                              identity=ident)
                        return ins
                    P.op("pe", trp, reads=[pcb, constb], writes=[pbb[pbk]])
                    src3 = PB[pbk][:, 0:n_here * 128].rearrange("p (a b) -> p a b", a=n_here)
                    dst3 = pT[:, pbk * 8:pbk * 8 + n_here, :]
                    if pbk == 0:
                        P.op("act", lambda e, src3=src3, dst3=dst3: e.activation(out=dst3, in_=src3, func=AF.Copy),
                             reads=[pbb[pbk]], writes=[pTb])
                    else:
                        P.op("dve", lambda e, src3=src3, dst3=dst3: e.tensor_copy(out=dst3, in_=src3),
                             reads=[pbb[pbk]], writes=[pTb])

                yield
                def mmpv(e):
                    ins = None
                    for kt in range(nkt):
                        ins = e.matmul(PF[5][:, 0:128], lhsT=pT[:, kt, :], rhs=vh[:, kt, :], start=(kt == 0),
                                       stop=(kt == nkt - 1))
                    return ins
                P.op("pe", mmpv, reads=[pTb, vhb], writes=[pfb[5]])
                P.op("dve", lambda e: e.tensor_scalar(out=o_bf, in0=PF[5][:, 0:128], scalar1=stn[:, 3:4], scalar2=None,
                                                      op0=ALU.mult), reads=[pfb[5], stnb], writes=[obfb])
                P.op("pe", lambda e: e.transpose(out=PB[1][:, 896:1024], in_=o_bf, identity=ident),
                     reads=[obfb, constb], writes=[pbb[1]])
                P.op("act", lambda e, h=h: e.activation(out=oT[:, h, tq], in_=PB[1][:, 896:1024], func=AF.Copy),
                     reads=[pbb[1]], writes=[oTb[t]])

            def drain(g):
                if g is not None:
                    for _ in g:
                        pass

            def step(g):
                if g is not None:
                    next(g, None)

            prevc = None
            for t in range(nt1 if sub1 >= 3 else 0):
                ctx = {}
                ga = unitA(t, unit_ctr[0] % 2, ctx)
                unit_ctr[0] += 1
                gb_ = unitB(prevc) if prevc is not None else None
                step(ga)
                step(gb_)
                step(ga)
                drain(gb_)
                drain(ga)
                prevc = ctx
            if prevc is not None:
                drain(unitB(prevc))
        P.barrier()
        if sub1 <= 3:
            return
        wo = RX[:, 0:8192].rearrange("p (k c) -> p k c", k=8)
        wob = Buf()
        P.dma("sp", "wo", lambda e: e.dma_start(out=wo, in_=wo1_s.rearrange("(k p) c -> p k c", p=128)),
              reads=[cvb["wo1"]], writes=[wob])
        gpost, gpostb = load_gain(5)
        tmp_view = view(RS, 0, [2, 1024], F32)
        tmp_bufs = [Buf(), Buf()]
        out_proj(oT, lambda t: [oTb[t]], 8, wo, wob, gpost, gpostb, tmp_view, tmp_bufs)
        P.barrier()

    P.dma("sp", "const", lambda e: e.dma_start(out=cmask[:, :], in_=cmask_d), writes=[constb])
    P.dma("pool", "constb", lambda e: e.dma_start(out=cbf[:, :], in_=cbf_d), writes=[constb])
    P.barrier()
    for h in range(4):
        convert("w0h_%d" % h, w0h_d[h], w0h_s[h], 2)
    convert("wo0", wo0_d, wo0_s, 2)
    for j in range(8):
        convert("wu0_%d" % j, wu_d[0][j], wu_s[0][j], 1)
        convert("wd0_%d" % j, wd_d[0][j], wd_s[0][j], 1)
    if stop >= 3:
        convert("w1h", w1h_d, w1h_s, 3)
        convert("wo1", wo1_d, wo1_s, 1)
    if stop >= 4:
        for j in range(8):
            convert("wu1_%d" % j, wu_d[1][j], wu_s[1][j], 1)
            convert("wd1_%d" % j, wd_d[1][j], wd_s[1][j], 1)


    def dump(s):
        for t in range(NT):
            P.dma("sp", "outd%d" % t, lambda e, t=t: e.dma_start(out=out_d[s, t * 128:(t + 1) * 128, :], in_=hview[:, t, :]),
                  reads=[hb[t]])

    import os as _os
    only_l1 = int(_os.environ.get("ONLYL1", 0))
    for s in range(nseq):
        if only_l1:
            for t in range(NT):
                P.dma("sp", "xh%d" % t, lambda e, t=t, s=s: e.dma_start(out=hview[:, t, :], in_=x_d[s, t * 128:(t + 1) * 128, :]),
                      writes=[hb[t]])
            if int(_os.environ.get("E4", 0)):
                P.barrier(final=True)
            layer1(s)
            dump(s)
            P.barrier()
            continue
        layer0(s, sub, nheads, ngroups)
        if stop == 1:
            dump(s)
            P.barrier()
            continue
        mlp(0, False, s)
        P.barrier()
        if stop == 2:
            dump(s)
            P.barrier()
            continue
        layer1(s)
        if stop == 3:
            dump(s)
            P.barrier()
            continue
        mlp(1, True, s)
        P.barrier()
    P.barrier(final=True)
    P.emit(nc, es)
    es.close()
    return nc


def _consts():
    pos = np.arange(S, dtype=np.float32)
    freq = (1.0 / (np.float32(10000.0) ** np.linspace(0.0, 1.0, 128, dtype=np.float32))).astype(np.float32)
    ang0 = freq[:, None] * pos[None, :]
    cs0 = np.stack([np.cos(ang0), np.sin(ang0)]).astype(np.float32)
    inv = (np.float32(500000.0) ** (-np.arange(0, 32, 2, dtype=np.float32) / np.float32(32))).astype(np.float32)
    ang1 = inv[:, None] * pos[None, :]
    c1 = np.concatenate([np.cos(ang1), np.cos(ang1), np.ones((96, S), np.float32)], 0)
    s1 = np.concatenate([np.sin(ang1), np.sin(ang1), np.zeros((96, S), np.float32)], 0)
    cs1 = np.stack([c1, s1]).astype(np.float32)
    gam = 1.0 - 2.0 ** (-5.0 - np.arange(4, dtype=np.float64))
    idx = np.arange(128, dtype=np.float64)
    cm = np.zeros((128, 4 * 128 + 4 * 128 + 4 + 2 * 256 + 64 + 8 + 8), np.float32)
    for h in range(4):
        diff = idx[None, :] - idx[:, None]
        m = np.where(diff >= 0, gam[h] ** np.maximum(diff, 0.0), 0.0) / 16.0
        cm[:, h * 128:(h + 1) * 128] = m
        cm[:, 512 + h * 128:512 + (h + 1) * 128] = (gam[h] ** (idx + 1.0))[None, :]
        cm[:, 1024 + h] = gam[h] ** (127.0 - idx) / 16.0
    q = np.arange(128)
    key = np.arange(256)
    cm[:, 1028:1028 + 256] = np.where(key[None, :] <= q[:, None], 0.0, -BIG)
    cm[:, 1028 + 256:1028 + 512] = np.where(key[None, :] <= q[:, None] + 128, 0.0, -BIG)
    pm = np.zeros((8, 8), np.float32)
    for b in range(8):
        pm[b, b:] = -1e30
    cm[:, 1540:1604] = pm.reshape(1, 64)
    cm[:, 1604:1612] = -0.5
    cm[:, 1612:1620] = 0.0
    cb = np.zeros((128, 256), np.float32)
    cb[:, 0:128] = np.eye(128)
    for f in range(16):
        cb[f + 16, 128 + f] = -1.0
        cb[f, 128 + f + 16] = 1.0
    return cs0, cs1, cm, cb


def _prep(inputs):
    f = lambda n: np.asarray(inputs[n], dtype=np.float32)
    w_in_0 = f("w_in_0")
    blocks = []
    perm = np.concatenate([np.arange(0, 256, 2), np.arange(1, 256, 2)])
    for h in range(4):
        qh = w_in_0[:, h * 256:(h + 1) * 256][:, perm]
        kh = w_in_0[:, 1024 + h * 256:1024 + (h + 1) * 256][:, perm]
        vh = w_in_0[:, 2048 + h * 512:2048 + (h + 1) * 512]
        gh = w_in_0[:, 4096 + h * 512:4096 + (h + 1) * 512]
        blocks.append(np.concatenate([qh, kh, vh, gh], axis=1))
    w0h = np.ascontiguousarray(np.stack(blocks))
    w_in_1 = f("w_in_1")
    w1h = np.ascontiguousarray(np.stack([np.concatenate([w_in_1[:, h * 128:(h + 1) * 128],
                                                         w_in_1[:, 1024 + h * 128:1024 + (h + 1) * 128],
                                                         w_in_1[:, 2048 + h * 128:2048 + (h + 1) * 128]], axis=1)
                                         for h in range(8)]))
    wu = [np.ascontiguousarray(f(n).reshape(1024, 8, 512).transpose(1, 0, 2)) for n in ("w_up_0", "w_up_1")]
    wd = [np.ascontiguousarray(f(n).reshape(8, 512, 1024)) for n in ("w_down_0", "w_down_1")]
    names = ["ln_mix_pre_0", "ln_mix_post_0", "ln_mlp_pre_0", "ln_mlp_post_0",
             "ln_mix_pre_1", "ln_mix_post_1", "ln_mlp_pre_1", "ln_mlp_post_1"]
    g8 = np.ascontiguousarray(np.stack([np.broadcast_to(f(n)[None, :], (128, D)) for n in names]))
    cs0, cs1, cm, cb = _consts()
    return {"w0h": w0h, "wo0": f("w_out_0"), "wu0": wu[0], "wu1": wu[1], "wd0": wd[0], "wd1": wd[1],
            "w1h": w1h, "wo1": f("w_out_1"), "g8": g8, "cs0": cs0, "cs1": cs1, "cmask": cm, "cbf": cb}


def kernel(**inputs):
    x = np.asarray(inputs["x"], dtype=np.float32)
    shared = _prep(inputs)
    nc = build_program(nseq=2, stop=4)
    in_maps = []
    for c in range(NCORES):
        m = dict(shared)
        m["x"] = np.ascontiguousarray(x[2 * c:2 * c + 2])
        in_maps.append(m)
    res = run_bass_kernel_spmd(nc, in_maps, core_ids=list(range(NCORES)))
    return np.concatenate([np.asarray(r["out"]) for r in res.results], axis=0).astype(np.float32)
```
